# Optimizing a Trainium2 kernel written in Bass

```python
import jax
import jax.numpy as jnp
from jax import lax
import numpy as np

D_MODEL = 2048
BATCH = 2
SEQ = 8192
DEPTH = 4

N_MIXERS = 2
N_CONV_LAYERS = (DEPTH + 1) // 2
N_GLA_LAYERS = DEPTH // 2
CONV_WIDTH = 31
GLA_HEADS = 4
GLA_DK = D_MODEL // 2
GLA_DV = D_MODEL
GLA_HEAD_K = GLA_DK // GLA_HEADS
GLA_HEAD_V = GLA_DV // GLA_HEADS
GLA_GATE_RANK = 16
GLA_TAU = 16.0
GLA_CHUNK = 64
GLA_IN_WIDTH = 2 * GLA_DK + 2 * GLA_DV + GLA_GATE_RANK
N_GROUPS = 4
EXPERTS_PER_GROUP = 8
N_EXPERTS = N_GROUPS * EXPERTS_PER_GROUP
TOP_K_IN_GROUP = 2
D_EXPERT = D_MODEL // 4
MOE_BLOCK = 128
N_ADA = 6
EPS = 1e-6

kernel_name = "hybrid_conv_gla_hmoe_adaln"


def rms_norm(x, gain):
    xf = x.astype(jnp.float32)
    y = xf * lax.rsqrt(jnp.mean(xf * xf, axis=-1, keepdims=True) + EPS)
    return (y * gain.astype(jnp.float32)).astype(x.dtype)


def conv_module(h, w_pw1, b_pw1, w_dw, b_dw, norm_g, w_pw2, b_pw2):
    u = h @ w_pw1 + b_pw1
    a, b = jnp.split(u, 2, axis=-1)
    u = a * jax.nn.sigmoid(b)
    u = lax.conv_general_dilated(
        u, w_dw[:, None, :].astype(u.dtype), window_strides=(1,),
        padding=[(CONV_WIDTH - 1, 0)], dimension_numbers=("NWC", "WIO", "NWC"),
        feature_group_count=D_MODEL) + b_dw
    u = jax.nn.silu(rms_norm(u, norm_g))
    return u @ w_pw2 + b_pw2


def gla_mixer(h, w_in, w_a2, b_a, head_g, w_o):
    B, S, _ = h.shape
    nc = S // GLA_CHUNK
    proj = h @ w_in
    q, k, v, r, a_lr = jnp.split(
        proj, [GLA_DK, 2 * GLA_DK, 2 * GLA_DK + GLA_DV, 2 * GLA_DK + 2 * GLA_DV], axis=-1)
    log_a = jax.nn.log_sigmoid((a_lr @ w_a2 + b_a).astype(jnp.float32)) / GLA_TAU

    def to_chunks(t, dh):
        return t.reshape(B, nc, GLA_CHUNK, GLA_HEADS, dh).transpose(1, 0, 3, 2, 4).astype(jnp.float32)

    qc = to_chunks(q, GLA_HEAD_K) * (GLA_HEAD_K ** -0.5)
    kc = to_chunks(k, GLA_HEAD_K)
    vc = to_chunks(v, GLA_HEAD_V)
    lac = to_chunks(log_a, GLA_HEAD_K)
    causal = jnp.tril(jnp.ones((GLA_CHUNK, GLA_CHUNK), dtype=bool))

    def step(state, inp):
        q_, k_, v_, la = inp
        cum = jnp.cumsum(la, axis=-2)
        cum_last = cum[..., -1:, :]
        q_dec = q_ * jnp.exp(cum)
        k_inv = k_ * jnp.exp(-cum)
        att = jnp.where(causal, jnp.einsum("bhik,bhjk->bhij", q_dec, k_inv), 0.0)
        o = (jnp.einsum("bhij,bhjv->bhiv", att, v_)
             + jnp.einsum("bhik,bhkv->bhiv", q_dec, state))
        k_end = k_ * jnp.exp(cum_last - cum)
        state = (jnp.exp(cum_last)[..., 0, :, None] * state
                 + jnp.einsum("bhjk,bhjv->bhkv", k_end, v_))
        return state, o

    state0 = jnp.zeros((B, GLA_HEADS, GLA_HEAD_K, GLA_HEAD_V), jnp.float32)
    _, o = lax.scan(step, state0, (qc, kc, vc, lac))
    o = rms_norm(o, head_g[:, None, :])
    o = o.transpose(1, 0, 3, 2, 4).reshape(B, S, GLA_DV).astype(h.dtype)
    return (jax.nn.silu(r) * o) @ w_o


def hier_moe(h, w_group, b_group, w_expert, b_expert, w13, w2):
    B, S, D = h.shape
    T = B * S
    A = T * TOP_K_IN_GROUP
    xt = h.reshape(T, D)
    g_prob = jax.nn.softmax((xt @ w_group).astype(jnp.float32) + b_group, axis=-1)
    g_val, g_idx = lax.top_k(g_prob, 1)
    e_logits = ((xt @ w_expert).astype(jnp.float32) + b_expert).reshape(T, N_GROUPS, EXPERTS_PER_GROUP)
    e_logits = jnp.take_along_axis(e_logits, g_idx[:, :, None], axis=1)[:, 0]
    e_val, e_idx = lax.top_k(jax.nn.softmax(e_logits, axis=-1), TOP_K_IN_GROUP)
    weight = g_val * (e_val / jnp.sum(e_val, axis=-1, keepdims=True))
    expert = g_idx * EXPERTS_PER_GROUP + e_idx

    flat_e = expert.reshape(A)
    flat_w = weight.reshape(A)
    flat_tok = jnp.repeat(jnp.arange(T, dtype=jnp.int32), TOP_K_IN_GROUP)
    order = jnp.argsort(flat_e)
    se = flat_e[order]
    tok_sorted = flat_tok[order]
    counts = jnp.bincount(flat_e, length=N_EXPERTS)
    starts = jnp.cumsum(counts) - counts
    padded = (counts + MOE_BLOCK - 1) // MOE_BLOCK * MOE_BLOCK
    pad_ends = jnp.cumsum(padded)
    pad_starts = pad_ends - padded
    dest = pad_starts[se] + jnp.arange(A, dtype=jnp.int32) - starts[se]
    n_blocks = A // MOE_BLOCK + N_EXPERTS
    xs = jnp.zeros((n_blocks * MOE_BLOCK, D), h.dtype).at[dest].set(xt[tok_sorted])
    block_expert = jnp.minimum(
        jnp.searchsorted(pad_ends, jnp.arange(n_blocks, dtype=jnp.int32) * MOE_BLOCK, side="right"),
        N_EXPERTS - 1)

    def expert_block(args):
        xb, e = args
        a, g = jnp.split(xb @ w13[e], 2, axis=-1)
        return (jax.nn.silu(a) * g) @ w2[e]

    ys = lax.map(expert_block, (xs.reshape(n_blocks, MOE_BLOCK, D), block_expert))
    ys = ys.reshape(n_blocks * MOE_BLOCK, D)
    contrib = ys[dest] * flat_w[order][:, None].astype(ys.dtype)
    out = jnp.zeros((T, D), ys.dtype).at[tok_sorted].add(contrib)
    return out.reshape(B, S, D)


def setup_inputs(seed: int = 0) -> dict:
    key = jax.random.key(seed)
    ks = jax.random.split(key, 25)
    L, Lc, Lg, D = DEPTH, N_CONV_LAYERS, N_GLA_LAYERS, D_MODEL

    def nrm(k, shape, s):
        return jax.random.normal(k, shape, jnp.float32) * s

    def gain(k, shape):
        return 1.0 + 0.01 * jax.random.normal(k, shape, jnp.float32)

    return {
        "x": nrm(ks[0], (BATCH, SEQ, D), 1.0),
        "c": nrm(ks[1], (BATCH, D), 1.0),
        "ada_w": nrm(ks[2], (L, D, N_ADA * D), 0.5 * D ** -0.5),
        "ada_b": nrm(ks[3], (L, N_ADA * D), 0.01),
        "mix_norm_g": gain(ks[4], (L, D)),
        "ffn_norm_g": gain(ks[5], (L, D)),
        "conv_w_pw1": nrm(ks[6], (Lc, D, 2 * D), D ** -0.5),
        "conv_b_pw1": nrm(ks[7], (Lc, 2 * D), 0.01),
        "conv_w_dw": nrm(ks[8], (Lc, CONV_WIDTH, D), CONV_WIDTH ** -0.5),
        "conv_b_dw": nrm(ks[9], (Lc, D), 0.01),
        "conv_norm_g": gain(ks[10], (Lc, D)),
        "conv_w_pw2": nrm(ks[11], (Lc, D, D), D ** -0.5),
        "conv_b_pw2": nrm(ks[12], (Lc, D), 0.01),
        "gla_w_in": nrm(ks[13], (Lg, D, GLA_IN_WIDTH), D ** -0.5),
        "gla_w_a2": nrm(ks[14], (Lg, GLA_GATE_RANK, GLA_DK), GLA_GATE_RANK ** -0.5),
        "gla_b_a": nrm(ks[15], (Lg, GLA_DK), 0.01),
        "gla_head_g": gain(ks[16], (Lg, GLA_HEADS, GLA_HEAD_V)),
        "gla_w_o": nrm(ks[17], (Lg, GLA_DV, D), GLA_DV ** -0.5),
        "moe_w_group": nrm(ks[18], (L, D, N_GROUPS), D ** -0.5),
        "moe_b_group": nrm(ks[19], (L, N_GROUPS), 0.01),
        "moe_w_expert": nrm(ks[20], (L, D, N_EXPERTS), D ** -0.5),
        "moe_b_expert": nrm(ks[21], (L, N_EXPERTS), 0.01),
        "moe_w13": nrm(ks[22], (L, N_EXPERTS, D, 2 * D_EXPERT), D ** -0.5),
        "moe_w2": nrm(ks[23], (L, N_EXPERTS, D_EXPERT, D), D_EXPERT ** -0.5),
        "final_norm_g": gain(ks[24], (D,)),
    }


def reference(x, c, ada_w, ada_b, mix_norm_g, ffn_norm_g,
              conv_w_pw1, conv_b_pw1, conv_w_dw, conv_b_dw, conv_norm_g, conv_w_pw2, conv_b_pw2,
              gla_w_in, gla_w_a2, gla_b_a, gla_head_g, gla_w_o,
              moe_w_group, moe_b_group, moe_w_expert, moe_b_expert, moe_w13, moe_w2,
              final_norm_g):
    cond = jax.nn.silu(c)
    for i in range(DEPTH):
        mod = cond @ ada_w[i] + ada_b[i]
        sh1, sc1, g1, sh2, sc2, g2 = [m[:, None, :] for m in jnp.split(mod, N_ADA, axis=-1)]
        h = rms_norm(x, mix_norm_g[i]) * (1.0 + sc1) + sh1
        j = i // N_MIXERS
        if i % N_MIXERS == 0:
            y = conv_module(h, conv_w_pw1[j], conv_b_pw1[j], conv_w_dw[j], conv_b_dw[j],
                            conv_norm_g[j], conv_w_pw2[j], conv_b_pw2[j])
        else:
            y = gla_mixer(h, gla_w_in[j], gla_w_a2[j], gla_b_a[j], gla_head_g[j], gla_w_o[j])
        x = x + g1 * y
        h = rms_norm(x, ffn_norm_g[i]) * (1.0 + sc2) + sh2
        x = x + g2 * hier_moe(h, moe_w_group[i], moe_b_group[i], moe_w_expert[i],
                              moe_b_expert[i], moe_w13[i], moe_w2[i])
    return rms_norm(x, final_norm_g)
```

```python
import numpy as np
import concourse.bass as bass
import concourse.mybir as mybir
from concourse.bass_utils import run_bass_kernel_spmd

F32 = mybir.dt.float32
BF16 = mybir.dt.bfloat16
I32 = mybir.dt.int32
U32 = mybir.dt.uint32
ALU = mybir.AluOpType
AF = mybir.ActivationFunctionType
AX = mybir.AxisListType

NCORES = 8
D = 2048
KC = 16
TOK = 2048
NT = 16
DEPTH = 4
CONVW = 31
HALO = 32
NEXP = 32
DEXP = 512
CAP = 512
EPS = 1e-6
GLA_DK = 1024
GLA_DV = 2048
GLA_R = 16
GLA_IN = 2 * GLA_DK + 2 * GLA_DV + GLA_R


class Buf:
    def __init__(self, name):
        self.name = name
        self.w = {}
        self.r = {}
        self.dma_sem = None
        self.dma_cnt = 0


class Eng:
    def __init__(self, name, h, sem):
        self.name = name
        self.h = h
        self.sem = sem
        self.cnt = 0
        self.waited = {}


class Prog:
    def __init__(self, nc):
        self.nc = nc
        self.engs = {}
        self.scopes = [[]]
        self.n_inst = 0
        self.n_wait = 0
        self.allbufs = []
        self.sem_pool = []
        self.sw_pool = []
        self.cc_pool = []
        self.sem_val = {}
        for name, h in (("pe", nc.tensor), ("act", nc.scalar), ("dve", nc.vector),
                        ("pool", nc.gpsimd), ("sp", nc.sync)):
            sem = nc.alloc_semaphore(name=f"sem_{name}")
            self.engs[name] = Eng(name, h, sem)
        self.nsem = 5

    def push(self):
        self.scopes.append([])

    def pop(self):
        self.barrier()
        sc = self.scopes.pop()
        for kind, obj in reversed(sc):
            if kind == "cm":
                obj.__exit__(None, None, None)
            elif kind == "swsem":
                self.sw_pool.append(obj)
            elif kind == "ccsem":
                self.cc_pool.append(obj)
            else:
                self.sem_pool.append(obj)

    def _enter(self, cm):
        t = cm.__enter__()
        self.scopes[-1].append(("cm", cm))
        return t

    def sbuf(self, name, shape, dt):
        self.uid = getattr(self, "uid", 0) + 1
        return self._enter(self.nc.sbuf_tensor(f"{name}_{self.uid}", list(shape), dt))

    def psum(self, name, shape, dt):
        self.uid = getattr(self, "uid", 0) + 1
        return self._enter(self.nc.psum_tensor(f"{name}_{self.uid}", list(shape), dt))

    def buf(self, name, dma=False, sw=False):
        b = Buf(name)
        if dma:
            pool = self.cc_pool if sw == "cc" else (self.sw_pool if sw else self.sem_pool)
            if pool:
                sem = pool.pop()
            else:
                sem = self.nc.alloc_semaphore(name=f"{'s' if sw else 'd'}sem{self.nsem}")
                self.nsem += 1
                self.sem_val[id(sem)] = 0
            b.dma_sem = sem
            b.dma_cnt = self.sem_val[id(sem)]
            b.sw = sw
            self.scopes[-1].append(("ccsem" if sw == "cc" else ("swsem" if sw else "sem"), sem))
        self.allbufs.append(b)
        return b

    def _deps(self, eng, reads, writes):
        need = {}

        def add(tok):
            sem, v = tok
            k = id(sem)
            if k not in need or need[k][1] < v:
                need[k] = (sem, v)
        for b in reads:
            for tok in b.w.values():
                add(tok)
        for b in writes:
            for tok in b.w.values():
                add(tok)
            for tok in b.r.values():
                add(tok)
        for k, (sem, v) in need.items():
            if sem is eng.sem and eng.name in ("pe", "sp"):
                continue
            if eng.waited.get(k, 0) >= v:
                continue
            eng.h.wait_ge(sem, v)
            self.n_wait += 1
            eng.waited[k] = v

    def _mark(self, tok, reads, writes):
        k = id(tok[0])
        for b in reads:
            b.r[k] = tok
        for b in writes:
            b.w = {k: tok}
            b.r = {}

    def op(self, engname, fn, reads=(), writes=()):
        eng = self.engs[engname]
        self._deps(eng, reads, writes)
        inst = fn(eng.h)
        eng.cnt += 1
        inst.then_inc(eng.sem, 1)
        self._mark((eng.sem, eng.cnt), reads, writes)
        self.n_inst += 1
        return inst

    def dma(self, engname, out, in_, side, reads=(), writes=(), **kw):
        return self.custom(engname, lambda e: e.dma_start(out=out, in_=in_, **kw), side, 16, reads, writes)

    def custom(self, engname, fn, side, inc, reads=(), writes=()):
        assert (engname == "pool") == bool(getattr(side, "sw", False)), f"semaphore class mismatch for {side.name}"
        eng = self.engs[engname]
        self._deps(eng, reads, writes)
        inst = fn(eng.h)
        side.dma_cnt += inc
        self.sem_val[id(side.dma_sem)] = side.dma_cnt
        inst.then_inc(side.dma_sem, inc)
        self._mark((side.dma_sem, side.dma_cnt), reads, writes)
        self.n_inst += 1
        return inst

    def barrier(self):
        for eng in self.engs.values():
            self._deps(eng, self.allbufs, self.allbufs)
        for b in self.allbufs:
            b.w = {}
            b.r = {}
        self.allbufs = [b for b in self.allbufs]
        for eng in self.engs.values():
            if eng.cnt > 20000:
                eng.sem = self.nc.alloc_semaphore(name=f"sem_{eng.name}_{self.nsem}")
                self.nsem += 1
                eng.cnt = 0

    def close(self):
        while len(self.scopes) > 1:
            self.pop()
        self.barrier()
        for kind, obj in reversed(self.scopes[0]):
            if kind == "cm":
                obj.__exit__(None, None, None)
        self.scopes = [[]]


class K:
    def __init__(self, nlayers=DEPTH, dbg=None):
        self.nlayers = nlayers
        self.dbg = dbg
        nc = bass.Bass("TRN2", target_bir_lowering=False)
        self.nc = nc
        self.P = Prog(nc)
        self.used_inputs = []
        self.Wfull = {}
        self.n_stg = 0
        self.stg_slots = [self.P.buf(f'stg_slot{i}', dma=True) for i in range(4)]
        self.declare_io()
        self.consts()

    INPUT_SPECS = {
        "x": [TOK, D], "c_col": [128, KC], "sel": [128, 16],
        "ada_b": [DEPTH, 6 * D], "mix_g": [DEPTH, D], "ffn_g": [DEPTH, D],
        "cb1c": [2, 128, 32], "cwdwc": [2, 128, KC * CONVW], "cbdwc": [2, 128, KC],
        "cngc": [2, 128, KC], "cb2": [2, D],
        "gw_a2": [2, GLA_R + 1, GLA_DK], "ghg": [2, GLA_DV],
        "mwr": [DEPTH, D, 36], "mbr": [DEPTH, 36],
        "fng": [D],
    }
    ALIASES = {"x_in": "x", "c_col": "c_col", "sel": "sel"}
    BIGW = {}
    for _l in range(DEPTH):
        BIGW[f"ada_w{_l}"] = (D, 6 * D, 2)
        BIGW[f"w13_{_l}"] = (NEXP * D, 2 * DEXP, 4)
        BIGW[f"w2_{_l}"] = (NEXP * DEXP, D, 2)
    for _j in range(2):
        BIGW[f"cw1_{_j}"] = (D, 2 * D, 1)
        BIGW[f"cw2_{_j}"] = (D, D, 1)
        BIGW[f"gw_in{_j}"] = (D, GLA_IN, 1)
        BIGW[f"gw_o{_j}"] = (GLA_DV, D, 1)

    def need(self, name):
        if name in self.Wfull:
            return self.Wfull[name]
        assert len(self.P.scopes) == 1, "need() must be first called at top scope"
        nc, P = self.nc, self.P
        rows, cols, ns = K.BIGW[name]
        R = rows // ns // NCORES
        inp = nc.dram_tensor(name, [ns, R, cols], F32, kind="ExternalInput").ap()
        self.used_inputs.append(name)
        stg = nc.dram_tensor(name + "_stg", [ns, R, cols], F32).ap()
        full = nc.dram_tensor(name + "_full", [rows, cols], F32).ap()
        trs = []
        for i in range(ns):
            slot = self.stg_slots[self.n_stg % len(self.stg_slots)]
            self.n_stg += 1
            bst = P.buf(f"{name}_st{i}")
            P.dma("sp", stg[i], inp[i], slot, writes=[bst, slot])
            bf = P.buf(f"{name}_f{i}", dma=True, sw="cc")
            P.custom("pool", lambda e, i=i: e.collective_compute(
                "AllGather", ALU.bypass, replica_groups=[list(range(NCORES))],
                ins=[stg[i].opt()], outs=[full[i * NCORES * R:(i + 1) * NCORES * R, :].opt()]),
                bf, 1, reads=[bst], writes=[bf])
            trs.append(bf)
        self.Wfull[name] = (full, trs, NCORES * R)
        return self.Wfull[name]

    def wap(self, name, row0=0):
        full, trs, blk = self.need(name)
        return full, trs[row0 // blk]

    def __getattr__(self, name):
        specs = K.INPUT_SPECS
        key = K.ALIASES.get(name, name)
        if key in specs:
            ap = self.nc.dram_tensor(key, list(specs[key]), F32, kind="ExternalInput").ap()
            self.used_inputs.append(key)
            setattr(self, name, ap)
            return ap
        raise AttributeError(name)

    def declare_io(self):
        nc = self.nc
        self.out = nc.dram_tensor("out", [TOK, D], F32, kind="ExternalOutput").ap()
        if self.dbg:
            self.dbg_out = nc.dram_tensor("dbg", list(self.dbg), F32, kind="ExternalOutput").ap()
        self.X = nc.dram_tensor("Xres", [TOK, D], F32).ap()
        self.bX = [self.P.buf(f"X{t}") for t in range(NT)]
        self.bXin = [self.P.buf(f"Xin{t}") for t in range(NT)]
        self.bOut = [self.P.buf(f"Out{t}") for t in range(NT)]
        self.hal_in = nc.dram_tensor("hal_in", [128, KC * HALO], BF16)
        self.hal_out = nc.dram_tensor("hal_out", [NCORES * 128, KC * HALO], BF16)

    def consts(self):
        P = self.P
        self.ident_bf = P.sbuf("ident_bf", [128, 128], BF16)
        self.ident_f = P.sbuf("ident_f", [128, 128], F32)
        self.ones_bf = P.sbuf("ones_bf", [128, 128], BF16)
        self.selt = P.sbuf("selt", [128, 16], F32)
        self.crep = P.sbuf("crep", [128, KC, 128], BF16)
        self.bconst = P.buf("consts", dma=True)
        bc = self.bconst
        P.op("pool", lambda e: e.memset(self.ident_bf[:], 1.0), writes=[bc])
        P.op("pool", lambda e: e.affine_select(out=self.ident_bf[:], in_=self.ident_bf[:], pattern=[[-1, 128]],
                                                compare_op=ALU.is_equal, fill=0.0, base=0, channel_multiplier=1),
             reads=[bc], writes=[bc])
        P.op("pool", lambda e: e.memset(self.ident_f[:], 1.0), writes=[bc])
        P.op("pool", lambda e: e.affine_select(out=self.ident_f[:], in_=self.ident_f[:], pattern=[[-1, 128]],
                                                compare_op=ALU.is_equal, fill=0.0, base=0, channel_multiplier=1),
             reads=[bc], writes=[bc])
        P.op("pool", lambda e: e.memset(self.ones_bf[:], 1.0), writes=[bc])
        P.dma("sp", self.selt[:], self.sel[:, :], bc, writes=[bc])
        P.push()
        cs = P.sbuf("cs", [128, KC], F32)
        P.dma("sp", cs[:], self.c_col[:, :], bc, writes=[bc])
        P.op("act", lambda e: e.activation(out=cs[:], in_=cs[:], func=AF.Silu), reads=[bc], writes=[bc])
        for kc in range(KC):
            P.op("dve", lambda e, kc=kc: e.tensor_scalar(out=self.crep[:, kc, :], in0=self.ones_bf[:],
                                                        scalar1=cs[:, kc:kc + 1], scalar2=None, op0=ALU.mult),
                 reads=[bc], writes=[bc])
        P.pop()
        self.PB = [P.psum(f"pb{i}", [128, 512], F32) for i in range(6)]
        self.bPB = [P.buf(f"pb{i}") for i in range(6)]
        self.TB = [P.psum(f"tb{i}", [128, 8, 128], BF16) for i in range(2)]
        self.bTB = [P.buf(f"tb{i}") for i in range(2)]

    def wblock(self, dst, bdst, wname, c0, ncols, nk=KC):
        full, trs, blk = self.need(wname)
        src = full[:, c0:c0 + ncols].rearrange("(kc p) n -> p kc n", p=128)
        self.P.dma("pool", dst, src, bdst, reads=trs, writes=[bdst])

    def ada_rows(self, l, specs):
        P = self.P
        P.push()
        wts = [P.sbuf(f"adaw{i}", [128, KC, 512], BF16) for i in range(2)]
        bws = [P.buf(f"adaw{i}", dma=True, sw=True) for i in range(2)]
        n = 0
        for (v, out, bout) in specs:
            P.dma("sp", out[:], self.ada_b[l, v * D:(v + 1) * D].partition_broadcast(128), bout, writes=[bout])
            for nb in range(4):
                s = n % 2
                self.wblock(wts[s][:], bws[s], f'ada_w{l}', v * D + nb * 512, 512)
                pb, bpb = self.PB[4 + s], self.bPB[4 + s]
                for kc in range(KC):
                    P.op("pe", lambda e, kc=kc, s=s, pb=pb: e.matmul(pb[:], lhsT=self.crep[:, kc, :], rhs=wts[s][:, kc, :],
                                                                      start=(kc == 0), stop=(kc == KC - 1)),
                         reads=[bws[s], self.bconst], writes=[bpb])
                P.op("dve", lambda e, nb=nb, pb=pb, out=out: e.tensor_tensor(out=out[:, nb * 512:(nb + 1) * 512], in0=pb[:],
                                                                           in1=out[:, nb * 512:(nb + 1) * 512], op=ALU.add),
                     reads=[bpb, bout], writes=[bout])
                n += 1
        P.pop()

    def norm_rows(self, l, which, A, bA, sh, bsh, g, bg):
        P = self.P
        base = 0 if which == 0 else 3
        self.ada_rows(l, [(base + 0, sh, bsh), (base + 1, A, bA), (base + 2, g, bg)])
        P.push()
        gt = P.sbuf("gain_t", [128, D], F32)
        bgt = P.buf("gain_t", dma=True)
        gsrc = self.mix_g if which == 0 else self.ffn_g
        P.dma("sp", gt[:], gsrc[l, :].partition_broadcast(128), bgt, writes=[bgt])
        P.op("dve", lambda e: e.scalar_tensor_tensor(out=A[:], in0=A[:], scalar=1.0, in1=gt[:], op0=ALU.add, op1=ALU.mult),
             reads=[bA, bgt], writes=[bA])
        P.pop()

    def rstd_col(self, ss, bss, n):
        P = self.P
        P.op("dve", lambda e: e.tensor_scalar(out=ss, in0=ss, scalar1=1.0 / n, scalar2=EPS, op0=ALU.mult, op1=ALU.add),
             reads=[bss], writes=[bss])
        P.op("act", lambda e: e.sqrt(out=ss, in_=ss), reads=[bss], writes=[bss])
        P.op("dve", lambda e: e.reciprocal(out=ss, in_=ss), reads=[bss], writes=[bss])

    def stage_norm_T(self, xsrc, bxsrc, A, bA, sh, bsh, HT, bHT):
        P = self.P
        P.push()
        xt = [P.sbuf(f"n_xt{i}", [128, D], F32) for i in range(2)]
        bxt = [P.buf(f"n_xt{i}", dma=True) for i in range(2)]
        hb = [P.sbuf(f"n_hb{i}", [128, D], BF16) for i in range(2)]
        bhb = [P.buf(f"n_hb{i}") for i in range(2)]
        junk = P.sbuf("n_junk", [128, D], BF16)
        bjunk = P.buf("n_junk")
        ss = [P.sbuf(f"n_ss{i}", [128, 1], F32) for i in range(2)]
        bss = [P.buf(f"n_ss{i}") for i in range(2)]
        for t in range(NT):
            s = t % 2
            P.dma("sp", xt[s][:], xsrc[t * 128:(t + 1) * 128, :], bxt[s], reads=[bxsrc[t]], writes=[bxt[s]])
            P.op("act", lambda e, s=s: e.activation(out=junk[:], in_=xt[s][:], func=AF.Square, accum_out=ss[s][:]),
                 reads=[bxt[s]], writes=[bjunk, bss[s]])
            self.rstd_col(ss[s][:], bss[s], D)
            P.op("dve", lambda e, s=s: e.scalar_tensor_tensor(out=xt[s][:], in0=xt[s][:], scalar=ss[s][:, 0:1], in1=A[:],
                                                              op0=ALU.mult, op1=ALU.mult),
                 reads=[bxt[s], bss[s], bA], writes=[bxt[s]])
            P.op("pool", lambda e, s=s: e.tensor_tensor(out=hb[s][:], in0=xt[s][:], in1=sh[:], op=ALU.add),
                 reads=[bxt[s], bsh], writes=[bhb[s]])
            for h in range(2):
                for k in range(8):
                    kc = h * 8 + k
                    P.op("pe", lambda e, s=s, h=h, k=k, kc=kc: e.transpose(out=self.TB[h][:, k, :], in_=hb[s][:, kc * 128:(kc + 1) * 128],
                                                                             identity=self.ident_bf[:]),
                         reads=[bhb[s], self.bconst], writes=[self.bTB[h]])
                eng = "act" if h == 0 else "dve"
                if eng == "act":
                    P.op("act", lambda e, h=h, t=t: e.copy(out=HT[:, h * 8:(h + 1) * 8, t * 128:(t + 1) * 128], in_=self.TB[h][:]),
                         reads=[self.bTB[h]], writes=[bHT[t]])
                else:
                    P.op("dve", lambda e, h=h, t=t: e.tensor_copy(out=HT[:, h * 8:(h + 1) * 8, t * 128:(t + 1) * 128], in_=self.TB[h][:]),
                         reads=[self.bTB[h]], writes=[bHT[t]])
        P.pop()

    def conv_layer(self, l, xsrc, bxsrc):
        P = self.P
        j = l // 2
        P.push()
        HT = P.sbuf("HT", [128, KC, TOK], BF16)
        bHT = [P.buf(f"HT{t}") for t in range(NT)]
        g = P.sbuf("rowG", [128, D], F32); bg = P.buf("rowG", dma=True)
        P.push()
        A = P.sbuf("rowA", [128, D], F32); bA = P.buf("rowA", dma=True)
        sh = P.sbuf("rowS", [128, D], F32); bsh = P.buf("rowS", dma=True)
        self.norm_rows(l, 0, A, bA, sh, bsh, g, bg)
        self.stage_norm_T(xsrc, bxsrc, A, bA, sh, bsh, HT, bHT)
        P.pop()

        P.push()
        UT = P.sbuf("UT", [128, KC, HALO + TOK], BF16)
        bUT = [[P.buf(f"UT{c}_{tb}") for tb in range(4)] for c in range(KC)]
        bUH = P.buf("UThalo")
        cols = P.sbuf("ccols", [128, 32 + KC * CONVW + KC + KC], F32)
        bcols = P.buf("ccols", dma=True)
        b1c = cols[:, 0:32]
        wdw = cols[:, 32:32 + KC * CONVW]
        bdw = cols[:, 32 + KC * CONVW:32 + KC * CONVW + KC]
        ngc = cols[:, 32 + KC * CONVW + KC:32 + KC * CONVW + 2 * KC]
        P.dma("sp", b1c, self.cb1c[j], bcols, writes=[bcols])
        P.dma("sp", wdw, self.cwdwc[j], bcols, writes=[bcols])
        P.dma("sp", bdw, self.cbdwc[j], bcols, writes=[bcols])
        P.dma("sp", ngc, self.cngc[j], bcols, writes=[bcols])

        P.push()
        wa = [P.sbuf(f"p1wa{i}", [128, KC, 128], BF16) for i in range(2)]
        wb = [P.sbuf(f"p1wb{i}", [128, KC, 128], BF16) for i in range(2)]
        bwa = [P.buf(f"p1wa{i}", dma=True, sw=True) for i in range(2)]
        bwb = [P.buf(f"p1wb{i}", dma=True, sw=True) for i in range(2)]
        sg = [P.sbuf(f"p1sg{i}", [128, 512], F32) for i in range(2)]
        bsg = [P.buf(f"p1sg{i}") for i in range(2)]
        n = 0
        for c in range(KC):
            s = c % 2
            self.wblock(wa[s][:], bwa[s], f'cw1_{j}', c * 128, 128)
            self.wblock(wb[s][:], bwb[s], f'cw1_{j}', D + c * 128, 128)
            for tb in range(4):
                q = n % 2
                pa, bpa = self.PB[q], self.bPB[q]
                pbb, bpbb = self.PB[2 + q], self.bPB[2 + q]
                rd = [bHT[4 * tb + i] for i in range(4)]
                for kc in range(KC):
                    P.op("pe", lambda e, kc=kc, s=s, tb=tb, pa=pa: e.matmul(pa[:], lhsT=wa[s][:, kc, :], rhs=HT[:, kc, tb * 512:(tb + 1) * 512],
                                                                            start=(kc == 0), stop=(kc == KC - 1)),
                         reads=[bwa[s]] + rd, writes=[bpa])
                for kc in range(KC):
                    P.op("pe", lambda e, kc=kc, s=s, tb=tb, pbb=pbb: e.matmul(pbb[:], lhsT=wb[s][:, kc, :], rhs=HT[:, kc, tb * 512:(tb + 1) * 512],
                                                                              start=(kc == 0), stop=(kc == KC - 1)),
                         reads=[bwb[s]] + rd, writes=[bpbb])
                P.op("act", lambda e, q=q, c=c, pbb=pbb: e.activation(out=sg[q][:], in_=pbb[:], func=AF.Sigmoid, bias=b1c[:, 16 + c:17 + c]),
                     reads=[bpbb, bcols], writes=[bsg[q]])
                P.op("dve", lambda e, q=q, c=c, tb=tb, pa=pa: e.scalar_tensor_tensor(
                    out=UT[:, c, HALO + tb * 512:HALO + (tb + 1) * 512], in0=pa[:], scalar=b1c[:, c:c + 1], in1=sg[q][:],
                    op0=ALU.add, op1=ALU.mult), reads=[bpa, bsg[q], bcols], writes=[bUT[c][tb]])
                n += 1
        P.pop()

        P.push()
        hall = P.sbuf("hall", [128, NCORES, KC * HALO], BF16)
        bhall = P.buf("hall", dma=True)
        hacc = P.sbuf("hacc", [128, KC * HALO], F32)
        bhacc = P.buf("hacc")
        bhin = P.buf("hal_in", dma=True)
        bhout = P.buf("hal_out", dma=True, sw="cc")
        allut3 = [bUT[c][3] for c in range(KC)]
        P.dma("sp", self.hal_in.ap().rearrange("p (c h) -> p c h", h=HALO), UT[:, :, TOK:TOK + HALO], bhin, reads=allut3, writes=[bhin])
        P.custom("pool", lambda e: e.collective_compute("AllGather", ALU.bypass, replica_groups=[list(range(NCORES))],
                                                         ins=[self.hal_in.ap().opt()], outs=[self.hal_out.ap().opt()]),
                 bhout, 1, reads=[bhin], writes=[bhout])
        P.dma("sp", hall[:], self.hal_out.ap().rearrange("(r p) f -> p r f", p=128), bhall, reads=[bhout], writes=[bhall])
        for r in range(NCORES):
            if r == 0:
                P.op("dve", lambda e: e.tensor_scalar(out=hacc[:], in0=hall[:, 0, :], scalar1=self.selt[:, 0:1], scalar2=None, op0=ALU.mult),
                     reads=[bhall, self.bconst], writes=[bhacc])
            else:
                P.op("dve", lambda e, r=r: e.scalar_tensor_tensor(out=hacc[:], in0=hall[:, r, :], scalar=self.selt[:, r:r + 1], in1=hacc[:],
                                                                  op0=ALU.mult, op1=ALU.add),
                     reads=[bhall, bhacc, self.bconst], writes=[bhacc])
        P.op("dve", lambda e: e.tensor_copy(out=UT[:, :, 0:HALO], in_=hacc[:].rearrange("p (c h) -> p c h", h=HALO)),
             reads=[bhacc], writes=[bUH])
        P.pop()

        VT = HT
        bVT = [[P.buf(f"VT{c}_{tb}") for tb in range(4)] for c in range(KC)]
        P.push()
        dg = [P.sbuf(f"cdg{i}", [128, CONVW, 128], BF16) for i in range(2)]
        bdg = [P.buf(f"cdg{i}") for i in range(2)]
        sq = [P.sbuf(f"csq{i}", [128, 512], BF16) for i in range(2)]
        bsq = [P.buf(f"csq{i}") for i in range(2)]
        n = 0
        for c in range(KC):
            s = c % 2
            for tap in range(CONVW):
                P.op("pool", lambda e, s=s, c=c, tap=tap: e.tensor_scalar(out=dg[s][:, tap, :], in0=self.ident_bf[:],
                                                                         scalar1=wdw[:, c * CONVW + tap:c * CONVW + tap + 1], scalar2=None, op0=ALU.mult),
                     reads=[bcols, self.bconst], writes=[bdg[s]])
            for tb in range(4):
                q = n % 2
                pc, bpc = self.PB[q], self.bPB[q]
                rd = [bUT[c][tb], bdg[s]] + ([bUT[c][tb - 1]] if tb > 0 else [bUH])
                for tap in range(CONVW):
                    o = HALO + tb * 512 - (CONVW - 1) + tap
                    P.op("pe", lambda e, s=s, c=c, tap=tap, o=o, pc=pc: e.matmul(pc[:], lhsT=dg[s][:, tap, :], rhs=UT[:, c, o:o + 512],
                                                                                 start=(tap == 0), stop=(tap == CONVW - 1)),
                         reads=rd, writes=[bpc])
                P.op("act", lambda e, c=c, tb=tb, pc=pc: e.activation(out=VT[:, c, tb * 512:(tb + 1) * 512], in_=pc[:], func=AF.Identity,
                                                                      bias=bdw[:, c:c + 1]),
                     reads=[bpc, bcols], writes=[bVT[c][tb]])
                P.op("act", lambda e, c=c, q=q, pc=pc: e.activation(out=sq[q][:], in_=pc[:], func=AF.Square, bias=bdw[:, c:c + 1]),
                     reads=[bpc, bcols], writes=[bsq[q]])
                P.op("pe", lambda e, q=q, tb=tb, c=c: e.matmul(self.PB[2 + tb][:], lhsT=self.ones_bf[:], rhs=sq[q][:],
                                                               start=(c == 0), stop=(c == KC - 1)),
                     reads=[bsq[q], self.bconst], writes=[self.bPB[2 + tb]])
                n += 1
        P.pop()
        P.push()
        rb = [P.sbuf(f"crb{i}", [128, 512], F32) for i in range(4)]
        brb = [P.buf(f"crb{i}") for i in range(4)]
        tmp = [P.sbuf(f"ctmp{i}", [128, 512], F32) for i in range(2)]
        btmp = [P.buf(f"ctmp{i}") for i in range(2)]
        for tb in range(4):
            P.op("dve", lambda e, tb=tb: e.tensor_scalar(out=rb[tb][:], in0=self.PB[2 + tb][:], scalar1=1.0 / D, scalar2=EPS,
                                                         op0=ALU.mult, op1=ALU.add), reads=[self.bPB[2 + tb]], writes=[brb[tb]])
            P.op("act", lambda e, tb=tb: e.sqrt(out=rb[tb][:], in_=rb[tb][:]), reads=[brb[tb]], writes=[brb[tb]])
            P.op("dve", lambda e, tb=tb: e.reciprocal(out=rb[tb][:], in_=rb[tb][:]), reads=[brb[tb]], writes=[brb[tb]])
        n = 0
        for tb in range(4):
            for c in range(KC):
                q = n % 2
                P.op("dve", lambda e, c=c, tb=tb, q=q: e.scalar_tensor_tensor(out=tmp[q][:], in0=VT[:, c, tb * 512:(tb + 1) * 512],
                                                                              scalar=ngc[:, c:c + 1], in1=rb[tb][:], op0=ALU.mult, op1=ALU.mult),
                     reads=[bVT[c][tb], brb[tb], bcols], writes=[btmp[q]])
                P.op("act", lambda e, c=c, tb=tb, q=q: e.activation(out=VT[:, c, tb * 512:(tb + 1) * 512], in_=tmp[q][:], func=AF.Silu),
                     reads=[btmp[q]], writes=[bVT[c][tb]])
                n += 1
        P.pop()
        P.pop()

        self.out_proj(VT, [[bVT[c][t // 4] for c in range(KC)] for t in range(NT)], f'cw2_{j}', self.cb2[j], g, bg, xsrc, bxsrc)
        P.pop()

    def out_proj(self, ST, bST_t, w2d, bias_row, g, bg, xsrc, bxsrc):
        P = self.P
        P.push()
        w = [P.sbuf(f"opw{i}", [128, KC, 512], BF16) for i in range(2)]
        bw = [P.buf(f"opw{i}", dma=True, sw=True) for i in range(2)]
        xt = [P.sbuf(f"opx{i}", [128, 512], F32) for i in range(3)]
        bxt = [P.buf(f"opx{i}", dma=True) for i in range(3)]
        yt = [P.sbuf(f"opy{i}", [128, 512], F32) for i in range(3)]
        byt = [P.buf(f"opy{i}") for i in range(3)]
        gb = P.sbuf("opgb", [128, D], F32)
        bgb = P.buf("opgb", dma=True)
        if bias_row is not None:
            P.dma("sp", gb[:], bias_row.partition_broadcast(128), bgb, writes=[bgb])
            P.op("dve", lambda e: e.tensor_tensor(out=gb[:], in0=gb[:], in1=g[:], op=ALU.mult), reads=[bgb, bg], writes=[bgb])
        n = 0
        for nb in range(4):
            s = nb % 2
            self.wblock(w[s][:], bw[s], w2d, nb * 512, 512)
            for t in range(NT):
                q = n % 2
                r = n % 3
                py, bpy = self.PB[q], self.bPB[q]
                P.dma("sp", xt[r][:], xsrc[t * 128:(t + 1) * 128, nb * 512:(nb + 1) * 512], bxt[r], reads=[bxsrc[t]], writes=[bxt[r]])
                for kc in range(KC):
                    P.op("pe", lambda e, kc=kc, s=s, t=t, py=py: e.matmul(py[:], lhsT=ST[:, kc, t * 128:(t + 1) * 128], rhs=w[s][:, kc, :],
                                                                          start=(kc == 0), stop=(kc == KC - 1)),
                         reads=[bw[s]] + bST_t[t], writes=[bpy])
                P.op("dve", lambda e, r=r, nb=nb, py=py: e.tensor_tensor(out=yt[r][:], in0=py[:], in1=g[:, nb * 512:(nb + 1) * 512], op=ALU.mult),
                     reads=[bpy, bg], writes=[byt[r]])
                if bias_row is not None:
                    P.op("pool", lambda e, r=r, nb=nb: e.tensor_tensor(out=yt[r][:], in0=yt[r][:], in1=gb[:, nb * 512:(nb + 1) * 512], op=ALU.add),
                         reads=[byt[r], bgb], writes=[byt[r]])
                P.op("pool", lambda e, r=r: e.tensor_tensor(out=xt[r][:], in0=xt[r][:], in1=yt[r][:], op=ALU.add),
                     reads=[byt[r], bxt[r]], writes=[bxt[r]])
                P.dma("sp", self.X[t * 128:(t + 1) * 128, nb * 512:(nb + 1) * 512], xt[r][:], bxt[r], reads=[bxt[r]], writes=[self.bX[t]])
                n += 1
        P.pop()

    def gla_consts(self):
        if hasattr(self, "tri_le_s"):
            return
        P = self.P
        nc = self.nc
        bc = self.bconst
        SC = -1.0 / 16.0
        self.tri_le_s = P.sbuf("tri_le_s", [128, 128], F32)
        self.tri_gt_s = P.sbuf("tri_gt_s", [128, 128], F32)
        self.mask_le = P.sbuf("mask_le", [128, 128], F32)
        self.ncol = P.sbuf("ncol", [128, 1], F32)
        P.op("pool", lambda e: e.memset(self.tri_le_s[:], SC), writes=[bc])
        P.op("pool", lambda e: e.affine_select(out=self.tri_le_s[:], in_=self.tri_le_s[:], pattern=[[1, 128]],
                                                compare_op=ALU.is_ge, fill=0.0, base=0, channel_multiplier=-1), reads=[bc], writes=[bc])
        P.op("pool", lambda e: e.memset(self.tri_gt_s[:], SC), writes=[bc])
        P.op("pool", lambda e: e.affine_select(out=self.tri_gt_s[:], in_=self.tri_gt_s[:], pattern=[[-1, 128]],
                                                compare_op=ALU.is_ge, fill=0.0, base=-1, channel_multiplier=1), reads=[bc], writes=[bc])
        P.op("pool", lambda e: e.memset(self.mask_le[:], 1.0), writes=[bc])
        P.op("pool", lambda e: e.affine_select(out=self.mask_le[:], in_=self.mask_le[:], pattern=[[1, 128]],
                                                compare_op=ALU.is_ge, fill=0.0, base=0, channel_multiplier=-1), reads=[bc], writes=[bc])
        P.op("pool", lambda e: e.memset(self.ncol[:], SC), writes=[bc])
        self.QT_d = nc.dram_tensor("QT_d", [GLA_DK, TOK], F32)
        self.KT_d = nc.dram_tensor("KT_d", [GLA_DK, TOK], F32)
        self.Ktm_d = nc.dram_tensor("Ktm_d", [TOK, GLA_DK], F32)
        self.V_d = nc.dram_tensor("V_d", [TOK, GLA_DV], BF16)
        self.SR_d = nc.dram_tensor("SR_d", [TOK, GLA_DV], F32)
        self.AL_d = nc.dram_tensor("AL_d", [32, TOK], F32)
        self.sx_in = nc.dram_tensor("sx_in", [128, 4096], F32)
        self.sx_out = nc.dram_tensor("sx_out", [NCORES * 128, 4096], F32)
        self.sa_in = nc.dram_tensor("sa_in", [128, 128], F32)
        self.sa_out = nc.dram_tensor("sa_out", [NCORES * 128, 128], F32)
        self.bQT = [[P.buf(f"QT{f}_{tb}") for tb in range(4)] for f in range(8)]
        self.bKT = [[P.buf(f"KT{f}_{tb}") for tb in range(4)] for f in range(8)]
        self.bKtm = [P.buf(f"Ktm{t}") for t in range(NT)]
        self.bV = [P.buf(f"V{t}") for t in range(NT)]
        self.bSR = [P.buf(f"SR{t}") for t in range(NT)]
        self.bAL = P.buf("AL")

    def gla_layer(self, l, xsrc, bxsrc, stop=None):
        P = self.P
        self.gla_consts()
        j = l // 2
        P.push()
        HT = P.sbuf("gHT", [128, KC, TOK], BF16)
        bHT = [P.buf(f"gHT{t}") for t in range(NT)]
        g = P.sbuf("g_rowG", [128, D], F32); bg = P.buf("g_rowG", dma=True)
        dbg_units = None
        if stop is not None and (stop.startswith("p1:") or stop.startswith("p2:")):
            dbg_units = int(stop[3:])
        if dbg_units is not None:
            return self._gla_rec(l, j, HT, bHT, g, bg, xsrc, bxsrc, stop, dbg_units)
        P.push()
        A = P.sbuf("g_rowA", [128, D], F32); bA = P.buf("g_rowA", dma=True)
        sh = P.sbuf("g_rowS", [128, D], F32); bsh = P.buf("g_rowS", dma=True)
        self.norm_rows(l, 0, A, bA, sh, bsh, g, bg)
        self.stage_norm_T(xsrc, bxsrc, A, bA, sh, bsh, HT, bHT)
        P.pop()
        W = f'gw_in{j}'

        P.push()
        wf = [P.sbuf(f"g_wf{i}", [128, KC, 128], BF16) for i in range(2)]
        bwf = [P.buf(f"g_wf{i}", dma=True, sw=True) for i in range(2)]
        ev = [P.sbuf(f"g_ev{i}", [128, 512], F32) for i in range(3)]
        bev = [P.buf(f"g_ev{i}", dma=True) for i in range(3)]
        alsb = P.sbuf("g_alsb", [32, TOK], F32); balsb = P.buf("g_alsb", dma=True)
        P.op("pool", lambda e: e.memset(alsb[:], 1.0), writes=[balsb])
        n = 0
        for f in range(17):
            s = f % 2
            ncol_ = 128 if f < 16 else GLA_R
            c0 = f * 128 if f < 16 else 2 * GLA_DK + 2 * GLA_DV
            self.wblock(wf[s][:, :, 0:ncol_], bwf[s], W, c0, ncol_)
            for tb in range(4):
                q = n % 2
                r = n % 3
                pb, bpb = self.PB[q], self.bPB[q]
                rd = [bHT[4 * tb + i] for i in range(4)]
                for kc in range(KC):
                    P.op("pe", lambda e, kc=kc, s=s, tb=tb, pb=pb, ncol_=ncol_: e.matmul(pb[0:ncol_, :], lhsT=wf[s][:, kc, 0:ncol_], rhs=HT[:, kc, tb * 512:(tb + 1) * 512],
                                                                                         start=(kc == 0), stop=(kc == KC - 1)),
                         reads=[bwf[s]] + rd, writes=[bpb])
                if f < 16:
                    dst = self.QT_d if f < 8 else self.KT_d
                    bd = (self.bQT if f < 8 else self.bKT)[f % 8][tb]
                    P.op("act", lambda e, r=r, pb=pb: e.copy(out=ev[r][:], in_=pb[:]), reads=[bpb], writes=[bev[r]])
                    P.dma("sp", dst[(f % 8) * 128:(f % 8 + 1) * 128, tb * 512:(tb + 1) * 512], ev[r][:], bev[r], reads=[bev[r]], writes=[bd])
                else:
                    P.op("act", lambda e, tb=tb, pb=pb: e.copy(out=alsb[0:GLA_R, tb * 512:(tb + 1) * 512], in_=pb[0:GLA_R, :]), reads=[bpb], writes=[balsb])
                n += 1
        P.dma("sp", self.AL_d[:, :], alsb[:], balsb, reads=[balsb], writes=[self.bAL])
        P.pop()
        P.push()
        wt = [P.sbuf(f"g_wt{i}", [128, KC, 512], BF16) for i in range(2)]
        bwt = [P.buf(f"g_wt{i}", dma=True, sw=True) for i in range(2)]
        evf = [P.sbuf(f"g_evf{i}", [128, 512], F32) for i in range(3)]
        bevf = [P.buf(f"g_evf{i}", dma=True) for i in range(3)]
        evb = [P.sbuf(f"g_evb{i}", [128, 512], BF16) for i in range(3)]
        bevb = [P.buf(f"g_evb{i}", dma=True) for i in range(3)]
        n = 0
        for blk in range(10):
            s = blk % 2
            self.wblock(wt[s][:], bwt[s], W, GLA_DK + blk * 512, 512)
            for t in range(NT):
                q = n % 2
                r = n % 3
                pb, bpb = self.PB[2 + q], self.bPB[2 + q]
                for kc in range(KC):
                    P.op("pe", lambda e, kc=kc, s=s, t=t, pb=pb: e.matmul(pb[:], lhsT=HT[:, kc, t * 128:(t + 1) * 128], rhs=wt[s][:, kc, :],
                                                                          start=(kc == 0), stop=(kc == KC - 1)),
                         reads=[bwt[s], bHT[t]], writes=[bpb])
                if blk < 2:
                    P.op("dve", lambda e, r=r, pb=pb: e.tensor_copy(out=evf[r][:], in_=pb[:]), reads=[bpb], writes=[bevf[r]])
                    P.dma("sp", self.Ktm_d[t * 128:(t + 1) * 128, blk * 512:(blk + 1) * 512], evf[r][:], bevf[r], reads=[bevf[r]], writes=[self.bKtm[t]])
                elif blk < 6:
                    P.op("dve", lambda e, r=r, pb=pb: e.tensor_copy(out=evb[r][:], in_=pb[:]), reads=[bpb], writes=[bevb[r]])
                    P.dma("sp", self.V_d[t * 128:(t + 1) * 128, (blk - 2) * 512:(blk - 1) * 512], evb[r][:], bevb[r], reads=[bevb[r]], writes=[self.bV[t]])
                else:
                    P.op("act", lambda e, r=r, pb=pb: e.activation(out=evf[r][:], in_=pb[:], func=AF.Silu), reads=[bpb], writes=[bevf[r]])
                    P.dma("sp", self.SR_d[t * 128:(t + 1) * 128, (blk - 6) * 512:(blk - 5) * 512], evf[r][:], bevf[r], reads=[bevf[r]], writes=[self.bSR[t]])
                n += 1
        P.pop()

        if stop == "proj":
            P.pop()
            return
        return self._gla_rec(l, j, HT, bHT, g, bg, xsrc, bxsrc, stop, None)

    def _gla_rec(self, l, j, HT, bHT, g, bg, xsrc, bxsrc, stop, dbg_units):
        P = self.P
        self._skip_xchg = False
        GT = HT
        bGT = [P.buf(f"GT{t}") for t in range(NT)]
        P.push()
        S = P.sbuf("g_S", [128, 8, 512], F32); bS = [P.buf(f"g_S{f}") for f in range(8)]
        Sb = P.sbuf("g_Sb", [128, 8, 512], BF16); bSb = [P.buf(f"g_Sb{f}") for f in range(8)]
        At = P.sbuf("g_At", [128, 8], F32); bAt = P.buf("g_At", dma=True)
        wa2 = P.sbuf("g_wa2", [32, GLA_DK], F32); bwa2 = P.buf("g_wa2", dma=True)
        hg = P.sbuf("g_hg", [128, GLA_DV], F32); bhg = P.buf("g_hg", dma=True)
        P.dma("sp", wa2[0:GLA_R + 1, :], self.gw_a2[j], bwa2, writes=[bwa2])
        P.dma("sp", hg[:], self.ghg[j, :].partition_broadcast(128), bhg, writes=[bhg])
        P.op("pool", lambda e: e.memset(S[:], 0.0), writes=bS)
        P.op("pool", lambda e: e.memset(At[:], 1.0), writes=[bAt])
        alc = [P.sbuf(f"g_alc{i}", [32, 128], F32) for i in range(2)]
        balc = [P.buf(f"g_alc{i}", dma=True) for i in range(2)]
        qT = [P.sbuf(f"g_qT{i}", [128, 2, 128], F32) for i in range(2)]
        kT = [P.sbuf(f"g_kT{i}", [128, 2, 128], F32) for i in range(2)]
        ktm = [P.sbuf(f"g_ktm{i}", [128, 256], F32) for i in range(2)]
        vu = [P.sbuf(f"g_vu{i}", [128, 512], BF16) for i in range(2)]
        sru = [P.sbuf(f"g_sru{i}", [128, 512], F32) for i in range(2)]
        bld = [P.buf(f"g_ld{i}", dma=True) for i in range(2)]
        l1 = P.sbuf("g_l1", [128, 256], F32); bl1 = P.buf("g_l1")
        tA = P.sbuf("g_tA", [128, 2, 128], F32); btA = P.buf("g_tA")
        tB = P.sbuf("g_tB", [128, 2, 128], F32); btB = P.buf("g_tB")
        tC = P.sbuf("g_tC", [128, 256], F32); btC = P.buf("g_tC")
        qd = P.sbuf("g_qd", [128, 2, 128], BF16); bqd = P.buf("g_qd")
        ki = P.sbuf("g_ki", [128, 2, 128], BF16); bki = P.buf("g_ki")
        ke = P.sbuf("g_ke", [128, 256], BF16); bke = P.buf("g_ke")
        dcol = P.sbuf("g_dcol", [128, 2], F32); bdcol = P.buf("g_dcol")
        attm = P.sbuf("g_attm", [128, 128], BF16); battm = P.buf("g_attm")
        og = P.sbuf("g_og", [128, 512], F32); bog = P.buf("g_og")
        ost = P.sbuf("g_ost", [128, 512], F32); bost = P.buf("g_ost")
        osum = P.sbuf("g_osum", [128, 512], F32); bosum = P.buf("g_osum")
        junk = P.sbuf("g_junk", [128, 512], BF16)
        ssq = P.sbuf("g_ssq", [128, 1], F32); bssq = P.buf("g_ssq")
        gated = [P.sbuf(f"g_gated{i}", [128, GLA_DV], BF16) for i in range(2)]
        bgated = [P.buf(f"g_gated{i}") for i in range(2)]
        PB, bPB = self.PB, self.bPB
        qtv = self.QT_d.ap().rearrange("(f p) t -> p f t", p=128)
        ktv = self.KT_d.ap().rearrange("(f p) t -> p f t", p=128)

        def unit(c, hd, full, n):
            cut = getattr(self, 'dbg_cut', 99)
            s = n % 2
            ca = c % 2
            f0 = hd * 2
            tb = c // 4
            if hd == 0:
                P.dma("sp", alc[ca][:], self.AL_d[:, c * 128:(c + 1) * 128], balc[ca], reads=[self.bAL], writes=[balc[ca]])
            P.dma("sp", ktm[s][:], self.Ktm_d[c * 128:(c + 1) * 128, hd * 256:(hd + 1) * 256], bld[s], reads=[self.bKtm[c]], writes=[bld[s]])
            P.dma("sp", vu[s][:], self.V_d[c * 128:(c + 1) * 128, hd * 512:(hd + 1) * 512], bld[s], reads=[self.bV[c]], writes=[bld[s]])
            if full:
                P.dma("sp", qT[s][:], qtv[:, f0:f0 + 2, c * 128:(c + 1) * 128], bld[s], reads=[self.bQT[f0][tb], self.bQT[f0 + 1][tb]], writes=[bld[s]])
                P.dma("sp", kT[s][:], ktv[:, f0:f0 + 2, c * 128:(c + 1) * 128], bld[s], reads=[self.bKT[f0][tb], self.bKT[f0 + 1][tb]], writes=[bld[s]])
                P.dma("sp", sru[s][:], self.SR_d[c * 128:(c + 1) * 128, hd * 512:(hd + 1) * 512], bld[s], reads=[self.bSR[c]], writes=[bld[s]])
            P.op("pe", lambda e: e.matmul(PB[0][:, 0:256], lhsT=alc[ca][0:GLA_R + 1, :], rhs=wa2[0:GLA_R + 1, hd * 256:(hd + 1) * 256], start=True, stop=True),
                 reads=[balc[ca], bwa2], writes=[bPB[0]])
            P.op("act", lambda e: e.activation(out=l1[:], in_=PB[0][:, 0:256], func=AF.Exp, scale=-1.0), reads=[bPB[0]], writes=[bl1])
            P.op("act", lambda e: e.activation(out=l1[:], in_=l1[:], func=AF.Ln, bias=1.0), reads=[bl1], writes=[bl1])
            P.op("pe", lambda e: e.matmul(PB[0][:, 256:512], lhsT=self.tri_gt_s[:], rhs=l1[:], start=True, stop=True),
                 reads=[bl1, self.bconst], writes=[bPB[0]])
            for kk in range(2):
                P.op("pe", lambda e, kk=kk: e.matmul(PB[1][:, 384 + kk:385 + kk], lhsT=l1[:, kk * 128:(kk + 1) * 128], rhs=self.ncol[:], start=True, stop=True),
                     reads=[bl1, self.bconst], writes=[bPB[1]])
            if full:
                for kk in range(2):
                    P.op("pe", lambda e, kk=kk: e.matmul(PB[1][:, kk * 128:(kk + 1) * 128], lhsT=l1[:, kk * 128:(kk + 1) * 128], rhs=self.tri_le_s[:], start=True, stop=True),
                         reads=[bl1, self.bconst], writes=[bPB[1]])
            P.op("act", lambda e: e.activation(out=dcol[:], in_=PB[1][:, 384:386], func=AF.Exp), reads=[bPB[1]], writes=[bdcol])
            P.op("act", lambda e: e.activation(out=tC[:], in_=PB[0][:, 256:512], func=AF.Exp), reads=[bPB[0]], writes=[btC])
            P.op("dve", lambda e: e.tensor_tensor(out=ke[:], in0=ktm[s][:], in1=tC[:], op=ALU.mult), reads=[bld[s], btC], writes=[bke])
            if full and cut >= 2:
                cumv = PB[1][:, 0:256].rearrange("p (k n) -> p k n", n=128)
                P.op("act", lambda e: e.activation(out=tA[:], in_=cumv, func=AF.Exp), reads=[bPB[1]], writes=[btA])
                P.op("act", lambda e: e.activation(out=tB[:], in_=cumv, func=AF.Exp, scale=-1.0), reads=[bPB[1]], writes=[btB])
                P.op("dve", lambda e: e.scalar_tensor_tensor(out=qd[:], in0=qT[s][:], scalar=0.0625, in1=tA[:], op0=ALU.mult, op1=ALU.mult),
                     reads=[bld[s], btA], writes=[bqd])
                P.op("dve", lambda e: e.tensor_tensor(out=ki[:], in0=kT[s][:], in1=tB[:], op=ALU.mult), reads=[bld[s], btB], writes=[bki])
            if full and cut >= 3:
                for kk in range(2):
                    P.op("pe", lambda e, kk=kk: e.matmul(PB[2][:, 0:128], lhsT=ki[:, kk, :], rhs=qd[:, kk, :], start=(kk == 0), stop=(kk == 1)),
                         reads=[bki, bqd], writes=[bPB[2]])
                P.op("dve", lambda e: e.tensor_tensor(out=attm[:], in0=PB[2][:, 0:128], in1=self.mask_le[:], op=ALU.mult),
                     reads=[bPB[2], self.bconst], writes=[battm])
            if full and cut >= 4:
                P.op("pe", lambda e: e.matmul(PB[3][:], lhsT=attm[:], rhs=vu[s][:], start=True, stop=True), reads=[battm, bld[s]], writes=[bPB[3]])
                for kk in range(2):
                    P.op("pe", lambda e, kk=kk: e.matmul(PB[1][:], lhsT=qd[:, kk, :], rhs=Sb[:, f0 + kk, :], start=(kk == 0), stop=(kk == 1)),
                         reads=[bqd, bSb[f0 + kk]], writes=[bPB[1]])
            if full and cut >= 5:
                P.op("act", lambda e: e.copy(out=ost[:], in_=PB[1][:]), reads=[bPB[1]], writes=[bost])
                P.op("dve", lambda e: e.tensor_tensor(out=osum[:], in0=PB[3][:], in1=ost[:], op=ALU.add), reads=[bPB[3], bost], writes=[bosum])
                P.op("act", lambda e: e.activation(out=junk[:], in_=osum[:], func=AF.Square, accum_out=ssq[:]), reads=[bosum], writes=[bssq])
                self.rstd_col(ssq[:], bssq, 512)
                P.op("dve", lambda e: e.scalar_tensor_tensor(out=og[:], in0=osum[:], scalar=ssq[:, 0:1], in1=hg[:, hd * 512:(hd + 1) * 512],
                                                             op0=ALU.mult, op1=ALU.mult), reads=[bosum, bssq, bhg], writes=[bog])
                P.op("pool", lambda e: e.tensor_tensor(out=gated[ca][:, hd * 512:(hd + 1) * 512], in0=og[:], in1=sru[s][:], op=ALU.mult),
                     reads=[bog, bld[s]], writes=[bgated[ca]])
            for kk in range(2):
                f = f0 + kk
                P.op("pe", lambda e, kk=kk: e.matmul(PB[4 + kk][:], lhsT=ke[:, kk * 128:(kk + 1) * 128], rhs=vu[s][:], start=True, stop=True),
                     reads=[bke, bld[s]], writes=[bPB[4 + kk]])
                P.op("dve", lambda e, kk=kk, f=f: e.scalar_tensor_tensor(out=S[:, f, :], in0=S[:, f, :], scalar=dcol[:, kk:kk + 1], in1=PB[4 + kk][:],
                                                                         op0=ALU.mult, op1=ALU.add), reads=[bS[f], bdcol, bPB[4 + kk]], writes=[bS[f]])
                if full:
                    P.op("act", lambda e, f=f: e.copy(out=Sb[:, f, :], in_=S[:, f, :]), reads=[bS[f]], writes=[bSb[f]])
            if not full:
                P.op("dve", lambda e: e.tensor_tensor(out=At[:, f0:f0 + 2], in0=At[:, f0:f0 + 2], in1=dcol[:], op=ALU.mult), reads=[bAt, bdcol], writes=[bAt])

        n = 0
        for c in range(NT):
            for hd in range(4):
                if dbg_units is not None and (n >= dbg_units or stop.startswith("p2:")):
                    continue
                if stop == "nop1":
                    continue
                unit(c, hd, False, n)
                n += 1
        if stop is not None and stop.startswith("p2:"):
            bdd = P.buf("dbgfill", dma=True)
            xi = self.x_in
            P.dma("sp", self.QT_d[:, :], xi[0:1024, :], bdd, writes=[bdd])
            P.dma("sp", self.KT_d[:, :], xi[1024:2048, :], bdd, writes=[bdd])
            P.dma("sp", self.Ktm_d[:, :], xi[:, 0:1024], bdd, writes=[bdd])
            P.dma("sp", self.SR_d[:, :], xi[:, :], bdd, writes=[bdd])
            P.dma("sp", self.AL_d[:, :], xi[0:32, :], bdd, writes=[bdd])
            bdv = P.buf("dbgfillv", dma=True, sw=True)
            vtmp = P.sbuf("dbg_vtmp", [128, D], BF16)
            for t in range(NT):
                P.dma("pool", vtmp[:], xi[t * 128:(t + 1) * 128, :], bdv, writes=[bdv])
                P.dma("sp", self.V_d[t * 128:(t + 1) * 128, :], vtmp[:], bdd, reads=[bdv], writes=[bdd, bdv])
            for b_ in [self.bAL] + self.bKtm + self.bV + self.bSR + [x for r in self.bQT for x in r] + [x for r in self.bKT for x in r]:
                b_.w = dict(bdd.w)
            for f in range(8):
                P.op("act", lambda e, f=f: e.copy(out=Sb[:, f, :], in_=S[:, f, :]), reads=[bS[f]], writes=[bSb[f]])
            n = 0
            for c in range(NT):
                ca = c % 2
                for hd in range(4):
                    if n >= dbg_units:
                        continue
                    unit(c, hd, True, n)
                    n += 1
                if n >= dbg_units and (c + 1) * 4 > dbg_units:
                    continue
                for h in range(2):
                    for k in range(8):
                        kc = h * 8 + k
                        P.op("pe", lambda e, ca=ca, h=h, k=k, kc=kc: e.transpose(out=self.TB[h][:, k, :], in_=gated[ca][:, kc * 128:(kc + 1) * 128],
                                                                                   identity=self.ident_bf[:]),
                             reads=[bgated[ca], self.bconst], writes=[self.bTB[h]])
                    P.op("act", lambda e, h=h, c=c: e.copy(out=GT[:, h * 8:(h + 1) * 8, c * 128:(c + 1) * 128], in_=self.TB[h][:]),
                         reads=[self.bTB[h]], writes=[bGT[c]])
        if stop == "pass1" or dbg_units is not None:
            P.pop(); P.pop()
            return
        self._skip_xchg = (stop in ("noxchg", "nop1"))
        P.push()
        bsx = P.buf("sx", dma=True)
        bsxo = P.buf("sxo", dma=True, sw="cc")
        if self._skip_xchg:
            P.pop()
            for f in range(8):
                P.op("act", lambda e, f=f: e.copy(out=Sb[:, f, :], in_=S[:, f, :]), reads=[bS[f]], writes=[bSb[f]])
            for c in range(NT):
                for hd in range(4):
                    unit(c, hd, True, c * 4 + hd)
            P.pop(); P.pop()
            return
        bsao = P.buf("sao", dma=True, sw="cc")
        atp = P.sbuf("g_atp", [128, 128], F32)
        P.op("dve", lambda e: e.memset(atp[:], 1.0), writes=[bsx])
        P.op("dve", lambda e: e.tensor_copy(out=atp[:, 0:8], in_=At[:]), reads=[bAt, bsx], writes=[bsx])
        P.dma("sp", self.sx_in[:, :], S[:].rearrange("p f v -> p (f v)"), bsx, reads=bS, writes=[bsx])
        P.dma("sp", self.sa_in[:, :], atp[:], bsx, reads=[bsx], writes=[bsx])
        P.custom("pool", lambda e: e.collective_compute("AllGather", ALU.bypass, replica_groups=[list(range(NCORES))],
                                                         ins=[self.sx_in.ap().opt()], outs=[self.sx_out.ap().opt()]),
                 bsxo, 1, reads=[bsx], writes=[bsxo])
        P.custom("pool", lambda e: e.collective_compute("AllGather", ALU.bypass, replica_groups=[list(range(NCORES))],
                                                         ins=[self.sa_in.ap().opt()], outs=[self.sa_out.ap().opt()]),
                 bsao, 1, reads=[bsx], writes=[bsao])
        sr_ = [P.sbuf(f"g_sxr{i}", [128, 4096 + 8], F32) for i in range(2)]
        bsr_ = [P.buf(f"g_sxr{i}", dma=True) for i in range(2)]
        coef = P.sbuf("g_coef", [128, 8], F32); bcoef = P.buf("g_coef")
        tmpS = P.sbuf("g_tmpS", [128, 512], F32); btmpS = P.buf("g_tmpS")
        P.op("pool", lambda e: e.memset(S[:], 0.0), reads=[bsx], writes=bS)
        for r in range(NCORES):
            u = r % 2
            P.dma("sp", sr_[u][:, 0:4096], self.sx_out[r * 128:(r + 1) * 128, :], bsr_[u], reads=[bsxo], writes=[bsr_[u]])
            P.dma("sp", sr_[u][:, 4096:4104], self.sa_out[r * 128:(r + 1) * 128, 0:8], bsr_[u], reads=[bsao], writes=[bsr_[u]])
            m = self.selt[:, 8 + r:9 + r]
            P.op("dve", lambda e, u=u, m=m: e.tensor_scalar(out=coef[:], in0=sr_[u][:, 4096:4104], scalar1=-1.0, scalar2=m, op0=ALU.add, op1=ALU.mult),
                 reads=[bsr_[u], self.bconst], writes=[bcoef])
            P.op("dve", lambda e: e.tensor_scalar(out=coef[:], in0=coef[:], scalar1=1.0, scalar2=None, op0=ALU.add), reads=[bcoef], writes=[bcoef])
            for f in range(8):
                P.op("dve", lambda e, u=u, m=m, f=f: e.tensor_scalar(out=tmpS[:], in0=sr_[u][:, f * 512:(f + 1) * 512], scalar1=m, scalar2=None, op0=ALU.mult),
                     reads=[bsr_[u], self.bconst], writes=[btmpS])
                P.op("dve", lambda e, f=f: e.scalar_tensor_tensor(out=S[:, f, :], in0=S[:, f, :], scalar=coef[:, f:f + 1], in1=tmpS[:], op0=ALU.mult, op1=ALU.add),
                     reads=[bS[f], bcoef, btmpS], writes=[bS[f]])
        for f in range(8):
            P.op("act", lambda e, f=f: e.copy(out=Sb[:, f, :], in_=S[:, f, :]), reads=[bS[f]], writes=[bSb[f]])
        P.pop()
        if stop == "xchg":
            P.pop(); P.pop()
            return
        n = 0
        for c in range(NT):
            ca = c % 2
            for hd in range(4):
                unit(c, hd, True, n)
                n += 1
            for h in range(2):
                for k in range(8):
                    kc = h * 8 + k
                    P.op("pe", lambda e, ca=ca, h=h, k=k, kc=kc: e.transpose(out=self.TB[h][:, k, :], in_=gated[ca][:, kc * 128:(kc + 1) * 128],
                                                                               identity=self.ident_bf[:]),
                         reads=[bgated[ca], self.bconst], writes=[self.bTB[h]])
                if h == 0:
                    P.op("act", lambda e, h=h, c=c: e.copy(out=GT[:, h * 8:(h + 1) * 8, c * 128:(c + 1) * 128], in_=self.TB[h][:]),
                         reads=[self.bTB[h]], writes=[bGT[c]])
                else:
                    P.op("dve", lambda e, h=h, c=c: e.tensor_copy(out=GT[:, h * 8:(h + 1) * 8, c * 128:(c + 1) * 128], in_=self.TB[h][:]),
                         reads=[self.bTB[h]], writes=[bGT[c]])
        P.pop()
        if stop == "pass2":
            P.pop()
            return
        self.out_proj(GT, [[bGT[t]] for t in range(NT)], f'gw_o{j}', None, g, bg, xsrc, bxsrc)
        P.pop()

    def moe_consts(self):
        if hasattr(self, "lts_bf"):
            return
        P = self.P
        bc = self.bconst
        self.lts_bf = P.sbuf("lts_bf", [128, 128], BF16)
        self.ebase = P.sbuf("ebase", [128, NEXP], F32)
        ebi = P.sbuf("ebase_i", [128, NEXP], I32)
        P.op("pool", lambda e: e.memset(self.lts_bf[:], 1.0), writes=[bc])
        P.op("pool", lambda e: e.affine_select(out=self.lts_bf[:], in_=self.lts_bf[:], pattern=[[1, 128]],
                                                compare_op=ALU.is_ge, fill=0.0, base=-1, channel_multiplier=-1),
             reads=[bc], writes=[bc])
        P.op("pool", lambda e: e.iota(ebi[:], pattern=[[CAP, NEXP]], base=0, channel_multiplier=0), writes=[bc])
        P.op("dve", lambda e: e.tensor_copy(out=self.ebase[:], in_=ebi[:]), reads=[bc], writes=[bc])
        nc = self.nc
        self.bc_reg = nc.gpsimd.to_reg(NEXP * CAP - 1)
        self.xs_d = nc.dram_tensor("xs_d", [NEXP * CAP, D], BF16)
        self.ys_d = nc.dram_tensor("ys_d", [NEXP * CAP, D], F32)
        self.bxs_d = P.buf("xs_d")
        self.bys_d = [P.buf(f"ys_d{e}") for e in range(NEXP)]

    def moe_layer(self, l, xsrc, bxsrc):
        P = self.P
        self.moe_consts()
        BIG = 30000.0
        P.push()
        g = P.sbuf("m_rowG", [128, D], F32); bg = P.buf("m_rowG", dma=True)
        desti = P.sbuf("m_dest", [128, 2 * NT], I32); bdest = [P.buf(f"m_dest{t}") for t in range(NT)]
        wts = P.sbuf("m_wts", [128, 2 * NT], F32); bwts = [P.buf(f"m_wts{t}") for t in range(NT)]

        P.push()
        A = P.sbuf("m_rowA", [128, D], F32); bA = P.buf("m_rowA", dma=True)
        sh = P.sbuf("m_rowS", [128, D], F32); bsh = P.buf("m_rowS", dma=True)
        self.norm_rows(l, 1, A, bA, sh, bsh, g, bg)
        wr = P.sbuf("m_wr", [128, KC, 36], F32); bwr = P.buf("m_wr", dma=True)
        brow = P.sbuf("m_br", [128, 36], F32)
        P.dma("sp", wr[:], self.mwr[l].rearrange("(kc p) n -> p kc n", p=128), bwr, writes=[bwr])
        P.dma("sp", brow[:], self.mbr[l, :].partition_broadcast(128), bwr, writes=[bwr])
        zt = P.sbuf("m_zero", [128, D], BF16); bzt = P.buf("m_zero", dma=True)
        P.op("pool", lambda e: e.memset(zt[:], 0.0), writes=[bzt])
        xsv = self.xs_d.ap().rearrange("(n p) d -> p n d", p=128)
        for i in range(NEXP * CAP // 128 // 16):
            P.dma("sp", xsv[:, i * 16:(i + 1) * 16, :], zt[:].unsqueeze(1).to_broadcast([128, 16, D]), bzt, reads=[bzt], writes=[self.bxs_d])
        carry = P.sbuf("m_carry", [128, NEXP], F32); bcarry = P.buf("m_carry")
        P.op("pool", lambda e: e.memset(carry[:], 0.0), writes=[bcarry])
        xt = [P.sbuf(f"m_xt{i}", [128, D], F32) for i in range(2)]
        bxt = [P.buf(f"m_xt{i}", dma=True) for i in range(2)]
        hb = [P.sbuf(f"m_hb{i}", [128, D], BF16) for i in range(2)]
        bhb = [P.buf(f"m_hb{i}", dma=True, sw=True) for i in range(2)]
        junk = P.sbuf("m_junk", [128, D], BF16); bjunk = P.buf("m_junk")
        hT = P.sbuf("m_hT", [128, KC, 128], F32); bhT = [P.buf(f"m_hT{i}") for i in range(4)]
        sm = P.sbuf("m_small", [128, 512], F32); bsm = P.buf("m_small")
        mk = P.sbuf("m_mask", [128, 64], BF16)
        o = 0

        def col(n):
            nonlocal o
            a = sm[:, o:o + n]
            o += n
            return a
        ss = col(1); lg = col(36); gmax = col(1); ngmax = col(1); gmask = col(4); gexp = col(4); gsum = col(1); gval = col(1)
        t1 = col(4); ml = col(32); m1 = col(1); mask1 = col(32); ml2 = col(32); m2 = col(1); mask2 = col(32)
        dl = col(1); e2 = col(1); den = col(1); w1 = col(1); w2 = col(1); msum = col(32); rank = col(32); tmp32 = col(32)
        d1 = col(1); d2 = col(1); r1 = col(1); r2 = col(1); ov = col(1)
        for t in range(NT):
            s = t % 2
            P.dma("sp", xt[s][:], xsrc[t * 128:(t + 1) * 128, :], bxt[s], reads=[bxsrc[t]], writes=[bxt[s]])
            P.op("act", lambda e, s=s: e.activation(out=junk[:], in_=xt[s][:], func=AF.Square, accum_out=ss),
                 reads=[bxt[s]], writes=[bjunk, bsm])
            self.rstd_col(ss, bsm, D)
            P.op("dve", lambda e, s=s: e.scalar_tensor_tensor(out=xt[s][:], in0=xt[s][:], scalar=ss, in1=A[:], op0=ALU.mult, op1=ALU.mult),
                 reads=[bxt[s], bsm, bA], writes=[bxt[s]])
            P.op("pool", lambda e, s=s: e.tensor_tensor(out=xt[s][:], in0=xt[s][:], in1=sh[:], op=ALU.add),
                 reads=[bxt[s], bsh], writes=[bxt[s]])
            P.op("act", lambda e, s=s: e.copy(out=hb[s][:], in_=xt[s][:]), reads=[bxt[s]], writes=[bhb[s]])
            for gq in range(4):
                pb, bpb = self.PB[gq], self.bPB[gq]
                for k in range(4):
                    kc = gq * 4 + k
                    P.op("pe", lambda e, s=s, k=k, kc=kc, pb=pb: e.transpose(out=pb[:, k * 128:(k + 1) * 128], in_=xt[s][:, kc * 128:(kc + 1) * 128],
                                                                             identity=self.ident_f[:]),
                         reads=[bxt[s], self.bconst], writes=[bpb])
                if gq % 2 == 0:
                    P.op("act", lambda e, gq=gq, pb=pb: e.copy(out=hT[:, gq * 4:(gq + 1) * 4, :], in_=pb[:].rearrange("p (k n) -> p k n", n=128)),
                         reads=[bpb], writes=[bhT[gq]])
                else:
                    P.op("dve", lambda e, gq=gq, pb=pb: e.tensor_copy(out=hT[:, gq * 4:(gq + 1) * 4, :], in_=pb[:].rearrange("p (k n) -> p k n", n=128)),
                         reads=[bpb], writes=[bhT[gq]])
            pl, bpl = self.PB[4], self.bPB[4]
            for kc in range(KC):
                P.op("pe", lambda e, kc=kc: e.matmul(pl[:, 0:36], lhsT=hT[:, kc, :], rhs=wr[:, kc, :], start=(kc == 0), stop=(kc == KC - 1)),
                     reads=[bhT[kc // 4], bwr], writes=[bpl])
            V = lambda fn, rd=(), wrt=(): P.op("dve", fn, reads=[bsm] + list(rd), writes=[bsm] + list(wrt))
            V(lambda e: e.tensor_tensor(out=lg, in0=pl[:, 0:36], in1=brow[:], op=ALU.add), rd=[bpl, bwr])
            V(lambda e: e.reduce_max(out=gmax, in_=lg[:, 0:4], axis=AX.X))
            V(lambda e: e.tensor_scalar(out=gmask, in0=lg[:, 0:4], scalar1=gmax, scalar2=None, op0=ALU.is_equal))
            V(lambda e: e.tensor_scalar(out=ngmax, in0=gmax, scalar1=-1.0, scalar2=None, op0=ALU.mult))
            P.op("act", lambda e: e.activation(out=gexp, in_=lg[:, 0:4], func=AF.Exp, bias=ngmax, accum_out=gsum), reads=[bsm], writes=[bsm])
            V(lambda e: e.reciprocal(out=gval, in_=gsum))
            V(lambda e: e.tensor_scalar(out=t1, in0=gmask, scalar1=BIG, scalar2=-BIG, op0=ALU.mult, op1=ALU.add))
            for gi in range(4):
                V(lambda e, gi=gi: e.tensor_scalar(out=ml[:, gi * 8:(gi + 1) * 8], in0=lg[:, 4 + gi * 8:12 + gi * 8],
                                                   scalar1=gmask[:, gi:gi + 1], scalar2=t1[:, gi:gi + 1], op0=ALU.mult, op1=ALU.add))
            V(lambda e: e.reduce_max(out=m1, in_=ml, axis=AX.X))
            V(lambda e: e.tensor_scalar(out=mask1, in0=ml, scalar1=m1, scalar2=None, op0=ALU.is_equal))
            V(lambda e: e.scalar_tensor_tensor(out=ml2, in0=mask1, scalar=-BIG, in1=ml, op0=ALU.mult, op1=ALU.add))
            V(lambda e: e.reduce_max(out=m2, in_=ml2, axis=AX.X))
            V(lambda e: e.tensor_scalar(out=mask2, in0=ml2, scalar1=m2, scalar2=None, op0=ALU.is_equal))
            V(lambda e: e.tensor_tensor(out=dl, in0=m2, in1=m1, op=ALU.subtract))
            P.op("act", lambda e: e.activation(out=e2, in_=dl, func=AF.Exp), reads=[bsm], writes=[bsm])
            V(lambda e: e.tensor_scalar(out=den, in0=e2, scalar1=1.0, scalar2=None, op0=ALU.add))
            V(lambda e: e.reciprocal(out=den, in_=den))
            V(lambda e, t=t: e.tensor_tensor(out=wts[:, 2 * t:2 * t + 1], in0=den, in1=gval, op=ALU.mult), wrt=[bwts[t]])
            V(lambda e, t=t: e.tensor_tensor(out=wts[:, 2 * t + 1:2 * t + 2], in0=wts[:, 2 * t:2 * t + 1], in1=e2, op=ALU.mult), rd=[bwts[t]], wrt=[bwts[t]])
            V(lambda e: e.tensor_tensor(out=msum, in0=mask1, in1=mask2, op=ALU.add))
            V(lambda e: e.tensor_copy(out=mk[:, 0:32], in_=msum))
            pr, bpr = self.PB[5], self.bPB[5]
            P.op("pe", lambda e: e.matmul(pr[:, 0:32], lhsT=self.lts_bf[:], rhs=mk[:, 0:32], start=True, stop=True),
                 reads=[bsm, self.bconst], writes=[bpr])
            P.op("pe", lambda e: e.matmul(pr[:, 64:96], lhsT=self.ones_bf[:], rhs=mk[:, 0:32], start=True, stop=True),
                 reads=[bsm, self.bconst], writes=[bpr])
            V(lambda e: e.tensor_tensor(out=rank, in0=pr[:, 0:32], in1=carry[:], op=ALU.add), rd=[bpr, bcarry])
            P.op("dve", lambda e: e.tensor_tensor(out=carry[:], in0=pr[:, 64:96], in1=carry[:], op=ALU.add), reads=[bpr, bsm, bcarry], writes=[bcarry])
            for kk, (mask, dd, rr) in enumerate(((mask1, d1, r1), (mask2, d2, r2))):
                V(lambda e, mask=mask, rr=rr: e.tensor_tensor(out=tmp32, in0=rank, in1=mask, op=ALU.mult))
                V(lambda e, rr=rr: e.reduce_sum(out=rr, in_=tmp32, axis=AX.X))
                V(lambda e, mask=mask: e.tensor_tensor(out=tmp32, in0=self.ebase[:], in1=mask, op=ALU.mult), rd=[self.bconst])
                V(lambda e, dd=dd: e.reduce_sum(out=dd, in_=tmp32, axis=AX.X))
                V(lambda e, rr=rr: e.tensor_scalar(out=ov, in0=rr, scalar1=float(CAP) - 0.5, scalar2=float(1 << 20), op0=ALU.is_gt, op1=ALU.mult))
                V(lambda e, dd=dd, rr=rr: e.tensor_tensor(out=dd, in0=dd, in1=rr, op=ALU.add))
                V(lambda e, dd=dd: e.tensor_tensor(out=dd, in0=dd, in1=ov, op=ALU.add))
                V(lambda e, dd=dd, t=t, kk=kk: e.tensor_copy(out=desti[:, 2 * t + kk:2 * t + kk + 1], in_=dd), wrt=[bdest[t]])
            for kk in range(2):
                P.custom("pool", lambda e, s=s, t=t, kk=kk: e.indirect_dma_start(
                    out=self.xs_d[:, :], out_offset=bass.IndirectOffsetOnAxis(ap=desti[:, 2 * t + kk:2 * t + kk + 1], axis=0),
                    in_=hb[s][:, :], in_offset=None, bounds_check=self.bc_reg, oob_is_err=False),
                    bhb[s], 16, reads=[bhb[s], bdest[t]], writes=[self.bxs_d])
        P.pop()

        P.push()
        w13b = [P.sbuf(f"m_w13_{i}", [128, KC, 2 * DEXP], BF16) for i in range(2)]
        bw13 = [P.buf(f"m_w13_{i}", dma=True, sw=True) for i in range(2)]
        w2b = [P.sbuf(f"m_w2_{i}", [128, 4, D], BF16) for i in range(2)]
        bw2 = [P.buf(f"m_w2_{i}", dma=True, sw=True) for i in range(2)]
        xtm = [P.sbuf(f"m_xtm{i}", [128, D], BF16) for i in range(2)]
        bxtm = [P.buf(f"m_xtm{i}", dma=True) for i in range(2)]
        xsT = P.sbuf("m_xsT", [128, KC, CAP], BF16)
        bxsT = [P.buf(f"m_xsT{i}") for i in range(CAP // 128)]
        hid = P.sbuf("m_hid", [128, 4, CAP], BF16)
        bhid = [P.buf(f"m_hid{i}") for i in range(4)]
        sa = [P.sbuf(f"m_sa{i}", [128, CAP], F32) for i in range(2)]
        bsa = [P.buf(f"m_sa{i}") for i in range(2)]
        ysb = [P.sbuf(f"m_ys{i}", [128, D], F32) for i in range(2)]
        bysb = [P.buf(f"m_ys{i}", dma=True) for i in range(2)]
        NB = CAP // 128

        def load_w(e):
            s = e % 2
            f13, t13 = self.wap(f"w13_{l}", e * D)
            f2, t2 = self.wap(f"w2_{l}", e * DEXP)
            src = f13[e * D:(e + 1) * D, :].rearrange("(kc p) n -> p kc n", p=128)
            P.dma("pool", w13b[s][:], src, bw13[s], reads=[t13], writes=[bw13[s]])
            P.dma("pool", w2b[s][:], f2[e * DEXP:(e + 1) * DEXP, :].rearrange("(kc p) n -> p kc n", p=128), bw2[s], reads=[t2], writes=[bw2[s]])
        load_w(0)
        nx = 0
        ny = 0
        for ex in range(NEXP):
            s = ex % 2
            if ex + 1 < NEXP:
                load_w(ex + 1)
            for blk in range(NB):
                u = nx % 2
                r0 = ex * CAP + blk * 128
                P.dma("sp", xtm[u][:], self.xs_d[r0:r0 + 128, :], bxtm[u], reads=[self.bxs_d], writes=[bxtm[u]])
                for h in range(2):
                    for k in range(8):
                        kc = h * 8 + k
                        P.op("pe", lambda e, u=u, h=h, k=k, kc=kc: e.transpose(out=self.TB[h][:, k, :], in_=xtm[u][:, kc * 128:(kc + 1) * 128],
                                                                                 identity=self.ident_bf[:]),
                             reads=[bxtm[u], self.bconst], writes=[self.bTB[h]])
                    if h == 0:
                        P.op("act", lambda e, h=h, blk=blk: e.copy(out=xsT[:, h * 8:(h + 1) * 8, blk * 128:(blk + 1) * 128], in_=self.TB[h][:]),
                             reads=[self.bTB[h]], writes=[bxsT[blk]])
                    else:
                        P.op("dve", lambda e, h=h, blk=blk: e.tensor_copy(out=xsT[:, h * 8:(h + 1) * 8, blk * 128:(blk + 1) * 128], in_=self.TB[h][:]),
                             reads=[self.bTB[h]], writes=[bxsT[blk]])
                nx += 1
            for jf in range(4):
                q = jf % 2
                pa, bpa = self.PB[q], self.bPB[q]
                pg, bpg = self.PB[2 + q], self.bPB[2 + q]
                for kc in range(KC):
                    P.op("pe", lambda e, kc=kc, s=s, jf=jf, pa=pa: e.matmul(pa[:, 0:CAP], lhsT=w13b[s][:, kc, jf * 128:(jf + 1) * 128], rhs=xsT[:, kc, :],
                                                                            start=(kc == 0), stop=(kc == KC - 1)),
                         reads=[bw13[s]] + bxsT, writes=[bpa])
                for kc in range(KC):
                    P.op("pe", lambda e, kc=kc, s=s, jf=jf, pg=pg: e.matmul(pg[:, 0:CAP], lhsT=w13b[s][:, kc, DEXP + jf * 128:DEXP + (jf + 1) * 128], rhs=xsT[:, kc, :],
                                                                            start=(kc == 0), stop=(kc == KC - 1)),
                         reads=[bw13[s]] + bxsT, writes=[bpg])
                P.op("act", lambda e, q=q, pa=pa: e.activation(out=sa[q][:], in_=pa[:, 0:CAP], func=AF.Silu), reads=[bpa], writes=[bsa[q]])
                P.op("dve", lambda e, q=q, jf=jf, pg=pg: e.tensor_tensor(out=hid[:, jf, :], in0=pg[:, 0:CAP], in1=sa[q][:], op=ALU.mult),
                     reads=[bpg, bsa[q]], writes=[bhid[jf]])
            for blk in range(NB):
                u = ny % 2
                for nb in range(4):
                    q = nb % 2
                    py, bpy = self.PB[4 + q], self.bPB[4 + q]
                    for fc in range(4):
                        P.op("pe", lambda e, fc=fc, s=s, blk=blk, nb=nb, py=py: e.matmul(py[:], lhsT=hid[:, fc, blk * 128:(blk + 1) * 128],
                                                                                         rhs=w2b[s][:, fc, nb * 512:(nb + 1) * 512],
                                                                                         start=(fc == 0), stop=(fc == 3)),
                             reads=[bw2[s]] + bhid, writes=[bpy])
                    if nb % 2 == 0:
                        P.op("act", lambda e, u=u, nb=nb, py=py: e.copy(out=ysb[u][:, nb * 512:(nb + 1) * 512], in_=py[:]), reads=[bpy], writes=[bysb[u]])
                    else:
                        P.op("dve", lambda e, u=u, nb=nb, py=py: e.tensor_copy(out=ysb[u][:, nb * 512:(nb + 1) * 512], in_=py[:]), reads=[bpy], writes=[bysb[u]])
                r0 = ex * CAP + blk * 128
                P.dma("sp", self.ys_d[r0:r0 + 128, :], ysb[u][:], bysb[u], reads=[bysb[u]], writes=[self.bys_d[ex]])
                ny += 1
        P.pop()

        P.push()
        y1 = [P.sbuf(f"m_y1_{i}", [128, D], F32) for i in range(2)]
        y2 = [P.sbuf(f"m_y2_{i}", [128, D], F32) for i in range(2)]
        by1 = [P.buf(f"m_y1_{i}", dma=True, sw=True) for i in range(2)]
        by2 = [P.buf(f"m_y2_{i}", dma=True, sw=True) for i in range(2)]
        xc = [P.sbuf(f"m_xc{i}", [128, D], F32) for i in range(2)]
        bxc = [P.buf(f"m_xc{i}", dma=True) for i in range(2)]
        for t in range(NT):
            s = t % 2
            P.dma("sp", xc[s][:], xsrc[t * 128:(t + 1) * 128, :], bxc[s], reads=[bxsrc[t]], writes=[bxc[s]])
            for kk, (yy, byy) in enumerate(((y1, by1), (y2, by2))):
                P.op("pool", lambda e, yy=yy, s=s: e.memset(yy[s][:], 0.0), writes=[byy[s]])
                P.custom("pool", lambda e, yy=yy, s=s, t=t, kk=kk: e.indirect_dma_start(
                    out=yy[s][:, :], out_offset=None, in_=self.ys_d[:, :],
                    in_offset=bass.IndirectOffsetOnAxis(ap=desti[:, 2 * t + kk:2 * t + kk + 1], axis=0),
                    bounds_check=self.bc_reg, oob_is_err=False),
                    byy[s], 16, reads=self.bys_d + [bdest[t]], writes=[byy[s]])
            P.op("dve", lambda e, s=s, t=t: e.tensor_scalar(out=y1[s][:], in0=y1[s][:], scalar1=wts[:, 2 * t:2 * t + 1], scalar2=None, op0=ALU.mult),
                 reads=[by1[s], bwts[t]], writes=[by1[s]])
            P.op("dve", lambda e, s=s, t=t: e.scalar_tensor_tensor(out=y1[s][:], in0=y2[s][:], scalar=wts[:, 2 * t + 1:2 * t + 2], in1=y1[s][:],
                                                                   op0=ALU.mult, op1=ALU.add),
                 reads=[by1[s], by2[s], bwts[t]], writes=[by1[s]])
            P.op("pool", lambda e, s=s: e.tensor_tensor(out=y1[s][:], in0=y1[s][:], in1=g[:], op=ALU.mult), reads=[by1[s], bg], writes=[by1[s]])
            P.op("pool", lambda e, s=s: e.tensor_tensor(out=xc[s][:], in0=xc[s][:], in1=y1[s][:], op=ALU.add), reads=[by1[s], bxc[s]], writes=[bxc[s]])
            P.dma("sp", self.X[t * 128:(t + 1) * 128, :], xc[s][:], bxc[s], reads=[bxc[s]], writes=[self.bX[t]])
        P.pop()
        P.pop()

    def final_norm(self, xsrc, bxsrc):
        P = self.P
        P.push()
        gr = P.sbuf("fn_g", [128, D], F32); bgr = P.buf("fn_g", dma=True)
        P.dma("sp", gr[:], self.fng.partition_broadcast(128), bgr, writes=[bgr])
        xt = [P.sbuf(f"fn_x{i}", [128, D], F32) for i in range(2)]
        bxt = [P.buf(f"fn_x{i}", dma=True) for i in range(2)]
        junk = P.sbuf("fn_junk", [128, D], BF16); bjunk = P.buf("fn_junk")
        ss = [P.sbuf(f"fn_ss{i}", [128, 1], F32) for i in range(2)]
        bss = [P.buf(f"fn_ss{i}") for i in range(2)]
        for t in range(NT):
            s = t % 2
            P.dma("sp", xt[s][:], xsrc[t * 128:(t + 1) * 128, :], bxt[s], reads=[bxsrc[t]], writes=[bxt[s]])
            P.op("act", lambda e, s=s: e.activation(out=junk[:], in_=xt[s][:], func=AF.Square, accum_out=ss[s][:]),
                 reads=[bxt[s]], writes=[bjunk, bss[s]])
            self.rstd_col(ss[s][:], bss[s], D)
            P.op("dve", lambda e, s=s: e.scalar_tensor_tensor(out=xt[s][:], in0=xt[s][:], scalar=ss[s][:, 0:1], in1=gr[:],
                                                              op0=ALU.mult, op1=ALU.mult),
                 reads=[bxt[s], bss[s], bgr], writes=[bxt[s]])
            P.dma("sp", self.out[t * 128:(t + 1) * 128, :], xt[s][:], bxt[s], reads=[bxt[s]], writes=[self.bOut[t]])
        P.pop()

    def prefetch(self, layers):
        for l in layers:
            jj = l // 2
            names = [f"ada_w{l}"] + ([f"cw1_{jj}", f"cw2_{jj}"] if l % 2 == 0 else [f"gw_in{jj}", f"gw_o{jj}"]) + [f"w13_{l}", f"w2_{l}"]
            for nm in names:
                self.need(nm)

    def full(self):
        self.prefetch(range(self.nlayers))
        src, bsrc = self.x_in, self.bXin
        for l in range(self.nlayers):
            if l % 2 == 0:
                self.conv_layer(l, src, bsrc)
            else:
                self.gla_layer(l, src, bsrc)
            src, bsrc = self.X, self.bX
            self.moe_layer(l, src, bsrc)
        self.final_norm(src, bsrc)

    def finish(self):
        global _LASTP
        _LASTP = self.P
        self.P.close()
        return self.nc


def build(nlayers=DEPTH, mode="full", dbg=None):
    k = K(nlayers, dbg)
    if mode == "conv_only":
        k.need("ada_w0"); k.need("cw1_0"); k.need("cw2_0")
        k.conv_layer(0, k.x_in, k.bXin)
        k.final_norm(k.X, k.bX)
    if mode.startswith("gla_cut"):
        k.dbg_cut = int(mode[7])
        mode = "gla_stop_nop1"
    if mode.startswith("gla_stop_"):
        if not (mode.startswith("gla_stop_p1:") or mode.startswith("gla_stop_p2:")):
            k.need("ada_w1"); k.need("gw_in0"); k.need("gw_o0")
        k.gla_layer(1, k.x_in, k.bXin, stop=mode[9:])
        k.final_norm(k.x_in, k.bXin)
    if mode == "gla_only":
        k.need("ada_w1"); k.need("gw_in0"); k.need("gw_o0")
        k.gla_layer(1, k.x_in, k.bXin)
        k.final_norm(k.X, k.bX)
    if mode == "full":
        k.full()
    if mode == "l0":
        k.prefetch([0])
        k.conv_layer(0, k.x_in, k.bXin)
        k.moe_layer(0, k.X, k.bX)
        k.final_norm(k.X, k.bX)
    return k.finish(), list(k.used_inputs)


def shared_inputs(inp):
    f = lambda a: np.ascontiguousarray(np.asarray(a, dtype=np.float32))
    d = {}
    d["ada_b"] = f(inp["ada_b"])
    src = {}
    for l in range(DEPTH):
        src[f"ada_w{l}"] = ("ada_w", l); src[f"w13_{l}"] = ("moe_w13", l); src[f"w2_{l}"] = ("moe_w2", l)
    for j in range(2):
        src[f"cw1_{j}"] = ("conv_w_pw1", j); src[f"cw2_{j}"] = ("conv_w_pw2", j)
        src[f"gw_in{j}"] = ("gla_w_in", j); src[f"gw_o{j}"] = ("gla_w_o", j)
    for nm, (key, idx) in src.items():
        def mk(nm=nm, key=key, idx=idx):
            rows, cols, ns = K.BIGW[nm]
            a = np.asarray(inp[key])[idx].reshape(ns, NCORES, rows // ns // NCORES, cols)
            return [f(a[:, r]) for r in range(NCORES)]
        d[nm] = mk
    d["mix_g"] = f(inp["mix_norm_g"]); d["ffn_g"] = f(inp["ffn_norm_g"])
    d["cb1c"] = f(np.asarray(inp["conv_b_pw1"]).reshape(2, 32, 128).transpose(0, 2, 1))
    d["cwdwc"] = f(np.asarray(inp["conv_w_dw"]).reshape(2, CONVW, KC, 128).transpose(0, 3, 2, 1).reshape(2, 128, KC * CONVW))
    d["cbdwc"] = f(np.asarray(inp["conv_b_dw"]).reshape(2, KC, 128).transpose(0, 2, 1))
    d["cngc"] = f(np.asarray(inp["conv_norm_g"]).reshape(2, KC, 128).transpose(0, 2, 1))
    d["cb2"] = f(inp["conv_b_pw2"])
    d["gw_a2"] = f(np.concatenate([np.asarray(inp["gla_w_a2"]), np.asarray(inp["gla_b_a"])[:, None, :]], axis=1))
    d["ghg"] = f(np.asarray(inp["gla_head_g"]).reshape(2, GLA_DV))
    d["mwr"] = f(np.concatenate([np.asarray(inp["moe_w_group"]), np.asarray(inp["moe_w_expert"])], axis=2))
    d["mbr"] = f(np.concatenate([np.asarray(inp["moe_b_group"]), np.asarray(inp["moe_b_expert"])], axis=1))
    d["fng"] = f(inp["final_norm_g"])
    return d


def core_inputs(inp, core):
    b, q = core // 4, core % 4
    d = {}
    d["x"] = np.ascontiguousarray(np.asarray(inp["x"])[b, q * TOK:(q + 1) * TOK], dtype=np.float32)
    d["c_col"] = np.ascontiguousarray(np.asarray(inp["c"])[b].reshape(KC, 128).T, dtype=np.float32)
    sel = np.zeros((128, 16), np.float32)
    if q > 0:
        sel[:, core - 1] = 1.0
    for r in range(NCORES):
        if r // 4 == b and r < core:
            sel[:, 8 + r] = 1.0
    d["sel"] = sel
    return d


def run(nc, used, inp, trace=False):
    sh = shared_inputs(inp)
    maps = []
    for c in range(NCORES):
        ci = core_inputs(inp, c)
        m = {}
        for k in used:
            if k in ci:
                m[k] = ci[k]
            else:
                if callable(sh[k]):
                    sh[k] = sh[k]()
                m[k] = sh[k][c] if isinstance(sh[k], list) else sh[k]
        maps.append(m)
    return run_bass_kernel_spmd(nc, maps, core_ids=list(range(NCORES)), trace=trace)


_CACHE = {}


def kernel(**inputs):
    if "prog" not in _CACHE:
        k = K()
        k.full()
        _CACHE["prog"] = (k.finish(), list(k.used_inputs))
    nc, used = _CACHE["prog"]
    res = run(nc, used, inputs)
    out = np.zeros((2, 4 * TOK, D), np.float32)
    for c in range(NCORES):
        out[c // 4, (c % 4) * TOK:(c % 4 + 1) * TOK] = res.results[c]["out"]
    return out
```
